# Optimizing a Trainium2 kernel written in Bass

```python
import math
import jax, jax.numpy as jnp
from jax import lax
import numpy as np

D_MODEL = 1024
BATCH = 16
SEQ = 4096
DEPTH = 1

CHUNK = 64
Q_BLOCK = 128
MAX_STREAM_OFFSET = 65536

LRU_WIDTH = D_MODEL
LRU_BLOCKS = 8
CONV_WIDTH = 4
LRU_C = 8.0

ATTN_HEADS = 8
ATTN_HEAD_DIM = D_MODEL // (2 * ATTN_HEADS)
ATTN_V_DIM = 2 * ATTN_HEAD_DIM
ATTN_QK_WIDTH = ATTN_HEADS * 2 * ATTN_HEAD_DIM
ATTN_WIDTH = ATTN_HEADS * ATTN_V_DIM
ROPE_THETA = 500000.0
ROT_DIMS = ATTN_HEAD_DIM // 4

MEM_TOKENS = 256
MEM_HEADS = 4
MEM_HEAD_DIM = 128
MEM_WIDTH = MEM_HEADS * MEM_HEAD_DIM

N_GROUPS = 4
EXPERTS_PER_GROUP = 8
N_EXPERTS = N_GROUPS * EXPERTS_PER_GROUP
TOP_K = 2
EXPERT_FF = 512
EXPERT_BLOCK = 128

IN_WIDTHS = (LRU_WIDTH, LRU_WIDTH, ATTN_QK_WIDTH, ATTN_QK_WIDTH, ATTN_WIDTH, D_MODEL, D_MODEL)
EPS = 1e-6
NEG_INF = -1e30

kernel_name = "hybrid_rglru_diffattn_hiermoe_block"


def rms_norm(x, g):
    xf = x.astype(jnp.float32)
    y = xf * lax.rsqrt(jnp.mean(xf * xf, axis=-1, keepdims=True) + EPS)
    return (y * g.astype(jnp.float32)).astype(x.dtype)


def rotary_tables(positions):
    half = ROT_DIMS // 2
    inv_freq = jnp.exp(-math.log(ROPE_THETA) * jnp.arange(half, dtype=jnp.float32) / half)
    ang = positions.astype(jnp.float32)[..., None] * inv_freq
    return jnp.cos(ang)[:, :, None, None, :], jnp.sin(ang)[:, :, None, None, :]


def rope_partial(x, cos, sin):
    half = ROT_DIMS // 2
    xr = x[..., :ROT_DIMS].astype(jnp.float32)
    x1, x2 = xr[..., :half], xr[..., half:]
    rot = jnp.concatenate([x1 * cos - x2 * sin, x2 * cos + x1 * sin], axis=-1)
    return jnp.concatenate([rot.astype(x.dtype), x[..., ROT_DIMS:]], axis=-1)


def causal_depthwise_conv(x, w, b):
    k = w.shape[0]
    y = lax.conv_general_dilated(x, w[:, None, :].astype(x.dtype), window_strides=(1,),
                                 padding=((k - 1, 0),), dimension_numbers=('NWC', 'WIO', 'NWC'),
                                 feature_group_count=x.shape[-1])
    return y + b


def block_diag_linear(x, w, b):
    bsz, s, c = x.shape
    nb, bw, _ = w.shape
    y = jnp.einsum('bsnc,ncd->bsnd', x.reshape(bsz, s, nb, bw), w).reshape(bsz, s, c)
    return y + b


def rg_lru(x, wa, ba, wx, bx, lam):
    r = jax.nn.sigmoid(block_diag_linear(x, wa, ba).astype(jnp.float32))
    i = jax.nn.sigmoid(block_diag_linear(x, wx, bx).astype(jnp.float32))
    log_a = -LRU_C * r * jax.nn.softplus(-lam.astype(jnp.float32))
    a = jnp.exp(log_a)
    b = jnp.sqrt(-jnp.expm1(2.0 * log_a)) * i * x.astype(jnp.float32)

    def combine(left, right):
        a_l, b_l = left
        a_r, b_r = right
        return a_l * a_r, a_r * b_l + b_r

    _, h = lax.associative_scan(combine, (a, b), axis=1)
    return h.astype(x.dtype)


def diff_attention(q, k, v, cos, sin, q_norm, k_norm, lam, lambda_init, subln):
    bsz, s, _ = q.shape
    q = q.reshape(bsz, s, ATTN_HEADS, 2, ATTN_HEAD_DIM)
    k = k.reshape(bsz, s, ATTN_HEADS, 2, ATTN_HEAD_DIM)
    v = v.reshape(bsz, s, ATTN_HEADS, ATTN_V_DIM)
    q = rope_partial(rms_norm(q, q_norm), cos, sin) * (ATTN_HEAD_DIM ** -0.5)
    k = rope_partial(rms_norm(k, k_norm), cos, sin)
    outs = []
    for blk in range(s // Q_BLOCK):
        q0, q1 = blk * Q_BLOCK, (blk + 1) * Q_BLOCK
        sc = jnp.einsum('bqhmd,bkhmd->bhmqk', q[:, q0:q1], k[:, :q1]).astype(jnp.float32)
        mask = (jnp.arange(q1) // CHUNK)[None, :] <= (jnp.arange(q0, q1) // CHUNK)[:, None]
        p = jax.nn.softmax(jnp.where(mask, sc, NEG_INF), axis=-1)
        w = p[:, :, 0] - lam * p[:, :, 1]
        outs.append(jnp.einsum('bhqk,bkhd->bqhd', w.astype(v.dtype), v[:, :q1]))
    o = jnp.concatenate(outs, axis=1)
    o = rms_norm(o, subln) * (1.0 - lambda_init)
    return o.reshape(bsz, s, ATTN_WIDTH)


def memory_cross_attention(x, mem, norm_cx, norm_mem, w_cq, w_ckv, cq_norm, ck_norm, w_co):
    bsz, s, _ = x.shape
    m = mem.shape[1]
    q = (rms_norm(x, norm_cx) @ w_cq).reshape(bsz, s, MEM_HEADS, MEM_HEAD_DIM)
    kv = rms_norm(mem, norm_mem) @ w_ckv
    k = kv[..., :MEM_WIDTH].reshape(bsz, m, MEM_HEADS, MEM_HEAD_DIM)
    v = kv[..., MEM_WIDTH:].reshape(bsz, m, MEM_HEADS, MEM_HEAD_DIM)
    q = rms_norm(q, cq_norm) * (MEM_HEAD_DIM ** -0.5)
    k = rms_norm(k, ck_norm)
    p = jax.nn.softmax(jnp.einsum('bqhd,bkhd->bhqk', q, k).astype(jnp.float32), axis=-1)
    o = jnp.einsum('bhqk,bkhd->bqhd', p.astype(v.dtype), v).reshape(bsz, s, MEM_WIDTH)
    return o @ w_co


def hierarchical_moe(h, w_group, b_group, w_router, b_router, w_gate_up, w_down):
    bsz, s, d = h.shape
    n = bsz * s
    a_tot = n * TOP_K
    hf = h.reshape(n, d)
    gp = jax.nn.softmax((hf @ w_group).astype(jnp.float32) + b_group.astype(jnp.float32), axis=-1)
    gval, gidx = lax.top_k(gp, 1)
    el = ((hf @ w_router).astype(jnp.float32) + b_router.astype(jnp.float32))
    el = el.reshape(n, N_GROUPS, EXPERTS_PER_GROUP)
    el = jnp.take_along_axis(el, gidx[:, :, None], axis=1)[:, 0]
    ev, eidx = lax.top_k(el, TOP_K)
    ew = jax.nn.softmax(ev, axis=-1) * gval
    expert_id = (gidx * EXPERTS_PER_GROUP + eidx).reshape(-1)
    token_id = jnp.repeat(jnp.arange(n, dtype=jnp.int32), TOP_K)
    weight = ew.reshape(-1)
    order = jnp.argsort(expert_id)
    sorted_e = expert_id[order]
    counts = jnp.bincount(expert_id, length=N_EXPERTS)
    padded = (counts + EXPERT_BLOCK - 1) // EXPERT_BLOCK * EXPERT_BLOCK
    start = jnp.cumsum(counts) - counts
    pend = jnp.cumsum(padded)
    pstart = pend - padded
    dest = pstart[sorted_e] + jnp.arange(a_tot) - start[sorted_e]
    p_rows = a_tot + N_EXPERTS * EXPERT_BLOCK
    n_blk = p_rows // EXPERT_BLOCK
    buf_tok = jnp.full((p_rows,), n, jnp.int32).at[dest].set(token_id[order])
    buf_w = jnp.zeros((p_rows,), jnp.float32).at[dest].set(weight[order])
    blk_e = jnp.minimum(jnp.searchsorted(pend, jnp.arange(n_blk) * EXPERT_BLOCK, side='right'),
                        N_EXPERTS - 1)
    h_pad = jnp.concatenate([hf, jnp.zeros((1, d), hf.dtype)], axis=0)
    xs = h_pad[buf_tok].reshape(n_blk, EXPERT_BLOCK, d)

    def expert_block(args):
        xb, e = args
        gu = xb @ w_gate_up[e]
        return (jax.nn.silu(gu[:, :EXPERT_FF]) * gu[:, EXPERT_FF:]) @ w_down[e]

    ys = lax.map(expert_block, (xs, blk_e)).reshape(p_rows, d)
    out = jnp.zeros((n + 1, d), ys.dtype).at[buf_tok].add(ys * buf_w[:, None].astype(ys.dtype))
    return out[:n].reshape(bsz, s, d)


def setup_inputs(seed: int = 0) -> dict:
    key = jax.random.key(seed)
    keys = iter(jax.random.split(key, 48))

    def normal(shape, scale):
        return jax.random.normal(next(keys), shape, jnp.float32) * scale

    def gain(n):
        return 1.0 + normal((DEPTH, n), 0.02)

    bw = LRU_WIDTH // LRU_BLOCKS
    u = jax.random.uniform(next(keys), (DEPTH, LRU_WIDTH), jnp.float32, 0.9, 0.999)
    p = u ** (1.0 / LRU_C)
    lru_lambda = jnp.log(p) - jnp.log1p(-p)
    offset = jax.random.randint(next(keys), (BATCH, 1), 0, MAX_STREAM_OFFSET, jnp.int32)
    positions = offset + jnp.arange(SEQ, dtype=jnp.int32)[None, :]
    return {
        "x": normal((BATCH, SEQ, D_MODEL), 1.0),
        "mem": normal((BATCH, MEM_TOKENS, D_MODEL), 1.0),
        "positions": positions,
        "norm_mix": gain(D_MODEL),
        "w_in": normal((DEPTH, D_MODEL, sum(IN_WIDTHS)), D_MODEL ** -0.5),
        "conv_w": normal((DEPTH, CONV_WIDTH, LRU_WIDTH), CONV_WIDTH ** -0.5),
        "conv_b": normal((DEPTH, LRU_WIDTH), 0.01),
        "lru_wa": normal((DEPTH, LRU_BLOCKS, bw, bw), bw ** -0.5),
        "lru_ba": normal((DEPTH, LRU_WIDTH), 0.01),
        "lru_wx": normal((DEPTH, LRU_BLOCKS, bw, bw), bw ** -0.5),
        "lru_bx": normal((DEPTH, LRU_WIDTH), 0.01),
        "lru_lambda": lru_lambda,
        "w_lru_o": normal((DEPTH, LRU_WIDTH, D_MODEL), LRU_WIDTH ** -0.5),
        "q_norm": gain(ATTN_HEAD_DIM),
        "k_norm": gain(ATTN_HEAD_DIM),
        "lambda_q1": normal((DEPTH, ATTN_HEAD_DIM), 0.1),
        "lambda_k1": normal((DEPTH, ATTN_HEAD_DIM), 0.1),
        "lambda_q2": normal((DEPTH, ATTN_HEAD_DIM), 0.1),
        "lambda_k2": normal((DEPTH, ATTN_HEAD_DIM), 0.1),
        "subln": gain(ATTN_V_DIM),
        "w_attn_o": normal((DEPTH, ATTN_WIDTH, D_MODEL), ATTN_WIDTH ** -0.5),
        "w_out": normal((DEPTH, D_MODEL, D_MODEL), D_MODEL ** -0.5),
        "norm_cx": gain(D_MODEL),
        "norm_mem": gain(D_MODEL),
        "w_cq": normal((DEPTH, D_MODEL, MEM_WIDTH), D_MODEL ** -0.5),
        "w_ckv": normal((DEPTH, D_MODEL, 2 * MEM_WIDTH), D_MODEL ** -0.5),
        "cq_norm": gain(MEM_HEAD_DIM),
        "ck_norm": gain(MEM_HEAD_DIM),
        "w_co": normal((DEPTH, MEM_WIDTH, D_MODEL), MEM_WIDTH ** -0.5),
        "norm_ffn": gain(D_MODEL),
        "w_group": normal((DEPTH, D_MODEL, N_GROUPS), D_MODEL ** -0.5),
        "b_group": normal((DEPTH, N_GROUPS), 0.01),
        "w_router": normal((DEPTH, D_MODEL, N_EXPERTS), D_MODEL ** -0.5),
        "b_router": normal((DEPTH, N_EXPERTS), 0.01),
        "w_gate_up": normal((DEPTH, N_EXPERTS, D_MODEL, 2 * EXPERT_FF), D_MODEL ** -0.5),
        "w_down": normal((DEPTH, N_EXPERTS, EXPERT_FF, D_MODEL), EXPERT_FF ** -0.5),
    }


def reference(x, mem, positions, norm_mix, w_in, conv_w, conv_b, lru_wa, lru_ba, lru_wx, lru_bx,
              lru_lambda, w_lru_o, q_norm, k_norm, lambda_q1, lambda_k1, lambda_q2, lambda_k2,
              subln, w_attn_o, w_out, norm_cx, norm_mem, w_cq, w_ckv, cq_norm, ck_norm, w_co,
              norm_ffn, w_group, b_group, w_router, b_router, w_gate_up, w_down):
    cos, sin = rotary_tables(positions)
    splits = np.cumsum(IN_WIDTHS)[:-1].tolist()
    for layer in range(DEPTH):
        lambda_init = 0.8 - 0.6 * math.exp(-0.3 * layer)
        h = rms_norm(x, norm_mix[layer])
        proj = h @ w_in[layer]
        lru_in, lru_gate, q, k, v, g_lru, g_attn = jnp.split(proj, splits, axis=-1)
        xc = causal_depthwise_conv(lru_in, conv_w[layer], conv_b[layer])
        hr = rg_lru(xc, lru_wa[layer], lru_ba[layer], lru_wx[layer], lru_bx[layer], lru_lambda[layer])
        y_lru = (jax.nn.gelu(lru_gate) * hr) @ w_lru_o[layer]
        lam = (jnp.exp(jnp.sum(lambda_q1[layer].astype(jnp.float32) * lambda_k1[layer].astype(jnp.float32)))
               - jnp.exp(jnp.sum(lambda_q2[layer].astype(jnp.float32) * lambda_k2[layer].astype(jnp.float32)))
               + lambda_init)
        o_attn = diff_attention(q, k, v, cos, sin, q_norm[layer], k_norm[layer], lam, lambda_init,
                                subln[layer])
        y_attn = o_attn @ w_attn_o[layer]
        mixed = jax.nn.sigmoid(g_lru) * y_lru + jax.nn.sigmoid(g_attn) * y_attn
        x = x + mixed @ w_out[layer]
        x = x + memory_cross_attention(x, mem, norm_cx[layer], norm_mem[layer], w_cq[layer],
                                       w_ckv[layer], cq_norm[layer], ck_norm[layer], w_co[layer])
        x = x + hierarchical_moe(rms_norm(x, norm_ffn[layer]), w_group[layer], b_group[layer],
                                 w_router[layer], b_router[layer], w_gate_up[layer], w_down[layer])
    return x
```

```python
import math
import numpy as np
from contextlib import ExitStack
import concourse.bass as bass
import concourse.mybir as mybir
from concourse.bass_utils import run_bass_kernel_spmd

F32 = mybir.dt.float32
BF16 = mybir.dt.bfloat16
I32 = mybir.dt.int32
ALU = mybir.AluOpType
AF = mybir.ActivationFunctionType
AX = mybir.AxisListType

D = 1024
EPS = 1e-6
N_CORES = 8


class Res:
    __slots__ = ("w", "r")

    def __init__(self):
        self.w = None
        self.r = {}


class Ring:
    def __init__(self, K, n, name):
        self.sems = [K.new_sem(f"{name}{i}") for i in range(n)]
        self.cnt = [0] * n
        self.i = 0

    def next(self):
        i = self.i
        self.i = (i + 1) % len(self.sems)
        return i


class Q:
    def __init__(self, K, eng, name, is_pe=False):
        self.eng = eng
        self.sem = K.new_sem("q_" + name)
        self.n = 0
        self.seen = {}
        self.is_pe = is_pe
        self.ring = None

    def need(self, tok):
        if tok is None:
            return
        sem, val = tok
        if self.is_pe and sem is self.sem:
            return
        k = id(sem)
        if self.seen.get(k, 0) >= val:
            return
        self.eng.wait_ge(sem, val)
        self.seen[k] = val

    def _deps(self, outs, ins):
        for t in ins:
            self.need(t.w)
        for t in outs:
            self.need(t.w)
            for tok in t.r.values():
                self.need(tok)

    def _mark(self, tok, outs, ins):
        k = id(tok[0])
        for t in ins:
            t.r[k] = tok
        for t in outs:
            t.w = tok
            t.r = {}

    def op(self, fn, outs=(), ins=(), inc=True):
        self._deps(outs, ins)
        inst = fn()
        if inc:
            self.n += 1
            inst.then_inc(self.sem, 1)
            tok = (self.sem, self.n)
        else:
            tok = (self.sem, self.n + 1)
        self._mark(tok, outs, ins)
        return tok

    def dma(self, out_ap, in_ap, outs=(), ins=(), fn=None):
        ring = self.ring
        i = ring.next()
        sem = ring.sems[i]
        self.need((sem, ring.cnt[i] * 16))
        self._deps(outs, ins)
        inst = self.eng.dma_start(out=out_ap, in_=in_ap) if fn is None else fn()
        inst.then_inc(sem, 16)
        ring.cnt[i] += 1
        tok = (sem, ring.cnt[i] * 16)
        self._mark(tok, outs, ins)
        return tok


class Kern:
    def __init__(self):
        self.nc = bass.Bass("TRN2", target_bir_lowering=False)
        self.es = ExitStack()
        nc = self.nc
        self.PE = Q(self, nc.tensor, "pe", is_pe=True)
        self.ACT = Q(self, nc.scalar, "act")
        self.DVE = Q(self, nc.vector, "dve")
        self.POOL = Q(self, nc.gpsimd, "pool")
        self.SP = Q(self, nc.sync, "sp")
        self.SP.ring = Ring(self, 30, "rsp")
        self.POOL.ring = Ring(self, 30, "rpl")
        self.ACT.ring = Ring(self, 8, "rac")
        self.engs = [self.PE, self.ACT, self.DVE, self.POOL, self.SP]
        self._n = 0
        self.scope = self.es

    def new_sem(self, name):
        return self.es.enter_context(self.nc.semaphore(name))

    def sb(self, shape, dt):
        self._n += 1
        return self.scope.enter_context(self.nc.sbuf_tensor(f"t{self._n}", list(shape), dt))

    def ps(self, shape, dt):
        self._n += 1
        return self.scope.enter_context(self.nc.psum_tensor(f"p{self._n}", list(shape), dt))

    def barrier(self):
        for e in self.engs:
            for e2 in self.engs:
                if e2 is not e:
                    e.need((e2.sem, e2.n))
            for q in (self.SP, self.POOL, self.ACT):
                for i, s in enumerate(q.ring.sems):
                    e.need((s, q.ring.cnt[i] * 16))


class Rot:
    def __init__(self, K, n, shape, dt, psum=False):
        self.t = [(K.ps(shape, dt) if psum else K.sb(shape, dt), Res()) for _ in range(n)]
        self.i = 0

    def get(self):
        r = self.t[self.i]
        self.i = (self.i + 1) % len(self.t)
        return r


PV_CONVW, PV_CONVB, PV_BA, PV_BX, PV_LAM, PV_QN, PV_KN, PV_CQN, PV_CKN, PV_N = 0, 32, 40, 48, 56, 64, 65, 66, 67, 68
GV_MIX, GV_CX, GV_MEM, GV_FFN, GV_SUBLN, GV_LAMV, GV_BRG, GV_N = 0, 1024, 2048, 3072, 4096, 4224, 4480, 4516
C_IDF, C_ONES, C_BLK, C_ROT, C_TRI, C_INVF, C_EOFF, C_N = 0, 128, 256, 384, 512, 640, 641, 673
ROT_DIMS, HALF, THETA = 16, 8, 500000.0
TWO_PI = 2.0 * math.pi
CW1 = 6.28125
CW2 = float(np.float32(TWO_PI - CW1))
CW3 = float(TWO_PI - CW1 - CW2)
MAGIC = 12582912.0


def build(nseq, S, cap, upto=99):
    T = nseq * S
    NG = T // 512
    GS = S // 512
    NT = T // 128
    NSLOT = 32 * cap
    K = Kern()
    nc = K.nc
    PE, ACT, DVE, POOL, SP = K.PE, K.ACT, K.DVE, K.POOL, K.SP
    V, A, G, TE = nc.vector, nc.scalar, nc.gpsimd, nc.tensor

    def din(name, shape, dt=F32):
        return nc.dram_tensor(name, list(shape), dt, kind="ExternalInput").ap()

    x_d = din("x", [T, D])
    mem_d = din("mem", [nseq * 256, D])
    pos_d = din("pos", [nseq, S], I32)
    w_in_d = din("w_in", [D, 7168])
    pvec_d = din("pvec", [128, PV_N])
    gvec_d = din("gvec", [128, GV_N])
    cst_d = din("cst", [128, C_N])
    wa_d = din("lru_wa", [8, 128, 128])
    wx_d = din("lru_wx", [8, 128, 128])
    wlo_d = din("w_lru_o", [D, D])
    wao_d = din("w_attn_o", [D, D])
    wout_d = din("w_out", [D, D])
    wcq_d = din("w_cq", [D, 512])
    wckv_d = din("w_ckv", [D, D])
    wco_d = din("w_co", [512, D])
    wrg_d = din("w_rg", [D, 36])
    wgu_d = din("w_gate_up", [32, D, D])
    wd_d = din("w_down", [32, 512, D])
    out_d = nc.dram_tensor("out", [T, D], F32, kind="ExternalOutput").ap()
    cnt_d = nc.dram_tensor("cnt", [128, 32], F32, kind="ExternalOutput").ap()
    hT_d = nc.dram_tensor("hT_s", [8, 128, T], BF16).ap()
    q_d = nc.dram_tensor("q_s", [8, 128, T], BF16).ap()
    k_d = nc.dram_tensor("k_s", [8, 128, T], BF16).ap()
    v_d = nc.dram_tensor("v_s", [T, D], BF16).ap()
    xs_d = nc.dram_tensor("xs_s", [NSLOT, D], BF16).ap()
    ys_d = nc.dram_tensor("ys_s", [NSLOT, D], BF16).ap()

    def kc(ap):
        return ap.rearrange("(kc p) n -> p kc n", p=128)

    cst = K.sb([128, C_N], F32); CST = Res()
    pvec = K.sb([128, PV_N], F32); PVEC = Res()
    identb = K.sb([128, 128], BF16); IDB = Res()
    onesb = K.sb([128, 128], BF16)
    neglam = K.sb([128, 1], F32); NEGLAM = Res()
    cl = K.sb([128, 16], F32); CL = Res()
    wout = K.sb([128, 8, D], BF16); WOUT = Res()
    SP.dma(cst[:], cst_d, outs=[CST])
    SP.dma(pvec[:], pvec_d, outs=[PVEC])
    POOL.dma(wout[:], kc(wout_d), outs=[WOUT])
    DVE.op(lambda: V.tensor_copy(identb[:], cst[:, C_IDF:C_IDF + 128]), outs=[IDB], ins=[CST])
    DVE.op(lambda: V.tensor_copy(onesb[:], cst[:, C_ONES:C_ONES + 128]), outs=[IDB], ins=[CST])
    blkb = K.sb([128, 128], BF16)
    rotb = K.sb([128, 128], BF16)
    DVE.op(lambda: V.tensor_copy(blkb[:], cst[:, C_BLK:C_BLK + 128]), outs=[IDB], ins=[CST])
    DVE.op(lambda: V.tensor_copy(rotb[:], cst[:, C_ROT:C_ROT + 128]), outs=[IDB], ins=[CST])
    identf = cst[:, C_IDF:C_IDF + 128]
    onesf = cst[:, C_ONES:C_ONES + 128]
    blkf = cst[:, C_BLK:C_BLK + 128]
    rotf = cst[:, C_ROT:C_ROT + 128]

    def pcol(i):
        return pvec[:, i:i + 1]

    with ExitStack() as sc:
        K.scope = sc
        lamv = K.sb([128, 256], F32); LV = Res()
        tmp = K.sb([128, 128], F32); TMP = Res()
        s2 = K.sb([128, 4], F32); S2 = Res()
        SP.dma(lamv[:], gvec_d[:, GV_LAMV:GV_LAMV + 256], outs=[LV])
        DVE.op(lambda: V.tensor_tensor(tmp[:, 0:64], lamv[:, 0:64], lamv[:, 64:128], op=ALU.mult), outs=[TMP], ins=[LV])
        DVE.op(lambda: V.reduce_sum(s2[:, 0:1], tmp[:, 0:64], axis=AX.X), outs=[S2], ins=[TMP])
        DVE.op(lambda: V.tensor_tensor(tmp[:, 64:128], lamv[:, 128:192], lamv[:, 192:256], op=ALU.mult), outs=[TMP], ins=[LV])
        DVE.op(lambda: V.reduce_sum(s2[:, 1:2], tmp[:, 64:128], axis=AX.X), outs=[S2], ins=[TMP])
        ACT.op(lambda: A.activation(s2[:, 2:4], s2[:, 0:2], AF.Exp), outs=[S2], ins=[S2])
        DVE.op(lambda: V.tensor_tensor(s2[:, 0:1], s2[:, 3:4], s2[:, 2:3], op=ALU.subtract), outs=[S2], ins=[S2])
        DVE.op(lambda: V.tensor_scalar(neglam[:], s2[:, 0:1], -0.2, None, op0=ALU.add), outs=[NEGLAM], ins=[S2])
        ACT.op(lambda: A.activation(tmp[:, 0:8], pvec[:, PV_LAM:PV_LAM + 8], AF.Exp, scale=-1.0), outs=[TMP], ins=[PVEC])
        ACT.op(lambda: A.activation(tmp[:, 8:16], tmp[:, 0:8], AF.Ln, bias=1.0), outs=[TMP], ins=[TMP])
        DVE.op(lambda: V.tensor_scalar(cl[:, 0:8], tmp[:, 8:16], -8.0, None, op0=ALU.mult), outs=[CL], ins=[TMP])
        DVE.op(lambda: V.tensor_scalar(cl[:, 8:16], tmp[:, 8:16], -16.0, None, op0=ALU.mult), outs=[CL], ins=[TMP])
        K.barrier()
    K.scope = K.es

    def rmsnorm_tile(xt, XT, gb, GB, hb, HB, junk, JK, st, ST, out_f32=None):
        ACT.op(lambda: A.activation(junk[:], xt, AF.Square, accum_out=st[:, 0:1]), outs=[JK, ST], ins=[XT])
        ACT.op(lambda: A.activation(st[:, 1:2], st[:, 0:1], AF.Ln, scale=1.0 / D, bias=EPS), outs=[ST], ins=[ST])
        ACT.op(lambda: A.activation(st[:, 2:3], st[:, 1:2], AF.Exp, scale=-0.5), outs=[ST], ins=[ST])
        DVE.op(lambda: V.scalar_tensor_tensor(hb, xt, st[:, 2:3], gb, op0=ALU.mult, op1=ALU.mult), outs=[HB], ins=[XT, ST, GB])

    def transpose8(src, SRC, pt, PT, dst, DST, eng, ncols=8, ident=None):
        idn = identb[:] if ident is None else ident
        for c in range(ncols):
            PE.op(lambda c=c: TE.transpose(pt[:, c, :], src[:, c * 128:(c + 1) * 128], idn), outs=[PT], ins=[SRC, IDB], inc=(c == ncols - 1))
        if eng is ACT:
            ACT.op(lambda: A.copy(dst, pt[:, 0:ncols, :]), outs=[DST], ins=[PT])
        else:
            DVE.op(lambda: V.tensor_copy(dst, pt[:, 0:ncols, :]), outs=[DST], ins=[PT])

    def acc_mm(ps_ap, PSR, pairs, ins):
        n = len(pairs)
        for i, (l, r) in enumerate(pairs):
            PE.op(lambda l=l, r=r, i=i: TE.matmul(ps_ap, l, r, start=(i == 0), stop=(i == n - 1)), outs=[PSR], ins=ins, inc=(i == n - 1))

    def phase_norm_T(src_d, gcol, dst_d, ntiles_total):
        with ExitStack() as sc:
            K.scope = sc
            gb = K.sb([128, D], F32); GB = Res()
            SP.dma(gb[:], gvec_d[:, gcol:gcol + D], outs=[GB])
            xr = Rot(K, 3, [128, D], F32)
            hbr = Rot(K, 2, [128, D], BF16)
            junk = K.sb([128, D], F32); JK = Res()
            str_ = Rot(K, 4, [128, 4], F32)
            ptr = Rot(K, 2, [128, 8, 128], BF16, psum=True)
            hTr = Rot(K, 2, [128, 8, 512], BF16)
            for g in range(ntiles_total // 4):
                hT, HT = hTr.get()
                for j in range(4):
                    t = g * 4 + j
                    xt, XT = xr.get()
                    SP.dma(xt[:], src_d[t * 128:(t + 1) * 128, :], outs=[XT])
                    hb, HB = hbr.get()
                    st, ST = str_.get()
                    rmsnorm_tile(xt[:], XT, gb[:], GB, hb[:], HB, junk, JK, st, ST)
                    pt, PT = ptr.get()
                    transpose8(hb, HB, pt, PT, hT[:, :, j * 128:(j + 1) * 128], HT, ACT if j % 2 else DVE)
                POOL.dma(dst_d[:, :, g * 512:(g + 1) * 512].rearrange("kc p t -> p kc t"), hT[:], ins=[HT])
            K.barrier()
        K.scope = K.es

    phase_norm_T(x_d, GV_MIX, hT_d, NT)

    z_d = nc.dram_tensor("z_s", [8, 128, T], BF16).ap()

    def post_mix(g, hT, HT, ybuild, wg, WG, sgr, zbuf, ZB, psr, xr, resid_d, mode="both", zlr=None):
        for fo in range(8):
            py, PY = psr.get()
            ybuild(fo, py, PY)
            pg, PG = psr.get()
            acc_mm(pg[:], PG, [(wg[:, k, fo * 128:(fo + 1) * 128], hT[:, k, :]) for k in range(8)], [WG, HT])
            sg, SG = sgr.get()
            ACT.op(lambda: A.activation(sg[:], pg[:], AF.Sigmoid), outs=[SG], ins=[PG])
            if mode == "add":
                zl, ZL = zlr.get()
                SP.dma(zl[:], z_d[fo, :, g * 512:(g + 1) * 512], outs=[ZL])
            DVE.op(lambda: V.tensor_tensor(zbuf[:, fo, :], sg[:], py[:], op=ALU.mult), outs=[ZB], ins=[SG, PY])
            if mode == "add":
                DVE.op(lambda: V.tensor_tensor(zbuf[:, fo, :], zbuf[:, fo, :], zl[:], op=ALU.add), outs=[ZB], ins=[ZB, ZL])
        if mode == "store":
            POOL.dma(z_d[:, :, g * 512:(g + 1) * 512].rearrange("kc p t -> p kc t"), zbuf[:], ins=[ZB])
            return
        for tt in range(4):
            t = g * 4 + tt
            xt, XT = xr.get()
            SP.dma(xt[:], resid_d[t * 128:(t + 1) * 128, :], outs=[XT])
            for hf in range(2):
                po, PO = psr.get()
                acc_mm(po[:], PO, [(zbuf[:, k, tt * 128:(tt + 1) * 128], wout[:, k, hf * 512:(hf + 1) * 512]) for k in range(8)], [ZB, WOUT])
                DVE.op(lambda: V.tensor_tensor(xt[:, hf * 512:(hf + 1) * 512], xt[:, hf * 512:(hf + 1) * 512], po[:], op=ALU.add), outs=[XT], ins=[XT, PO])
            POOL.dma(out_d[t * 128:(t + 1) * 128, :], xt[:], ins=[XT])

    m_d = nc.dram_tensor("m_s", [8, 128, T], BF16).ap()
    if upto >= 1:
        with ExitStack() as sc:
            K.scope = sc
            wl = K.sb([128, 8, 2048], BF16); WL = Res()
            wa = K.sb([128, 8, 128], BF16); WA = Res()
            wx = K.sb([128, 8, 128], BF16)
            POOL.dma(wl[:], kc(w_in_d[:, 0:2048]), outs=[WL])
            POOL.dma(wa[:], wa_d.rearrange("n c d -> c n d"), outs=[WA])
            POOL.dma(wx[:], wx_d.rearrange("n c d -> c n d"), outs=[WA])
            hTr = Rot(K, 2, [128, 8, 512], BF16)
            xin = K.sb([128, 8, 515], F32); XIN = Res()
            xc = K.sb([128, 8, 512], F32); XC = [Res() for _ in range(8)]
            xcb = K.sb([128, 8, 512], BF16); XCB = [Res() for _ in range(8)]
            rr = K.sb([128, 8, 512], F32); RR = [Res() for _ in range(8)]
            ii = K.sb([128, 8, 512], F32); II = [Res() for _ in range(8)]
            aa = K.sb([128, 8, 512], F32); AA = [Res() for _ in range(8)]
            CURS = [Res() for _ in range(8)]
            hst = K.sb([128, 8], F32); HST = Res()
            mbr = Rot(K, 2, [128, 8, 512], BF16)
            glr = Rot(K, 3, [128, 512], F32)
            psr = Rot(K, 8, [128, 512], F32, psum=True)
            cur = xin
            for g in range(NG):
                b, gi = divmod(g, GS)
                mbuf, MB = mbr.get()
                if gi == 0:
                    DVE.op(lambda: V.memset(cur[:, :, 0:3], 0.0), outs=CURS)
                    DVE.op(lambda: V.memset(hst[:], 0.0), outs=[HST])
                hT, HT = hTr.get()
                SP.dma(hT[:], hT_d[:, :, g * 512:(g + 1) * 512].rearrange("kc p t -> p kc t"), outs=[HT])
                for c in range(8):
                    ps, PS = psr.get()
                    acc_mm(ps[:], PS, [(wl[:, k, c * 128:(c + 1) * 128], hT[:, k, :]) for k in range(8)], [WL, HT])
                    ACT.op(lambda: A.copy(cur[:, c, 3:515], ps[:]), outs=[CURS[c]], ins=[PS])
                    cw = lambda j: pcol(PV_CONVW + c * 4 + j)
                    ACT.op(lambda: A.activation(xc[:, c, :], ps[:], AF.Identity, scale=cw(3), bias=pcol(PV_CONVB + c)), outs=[XC[c]], ins=[PS, PVEC])
                    for j in range(3):
                        DVE.op(lambda j=j: V.scalar_tensor_tensor(xc[:, c, :], cur[:, c, j:j + 512], cw(j), xc[:, c, :], op0=ALU.mult, op1=ALU.add), outs=[XC[c]], ins=[CURS[c], PVEC, XC[c]])
                    POOL.op(lambda: G.tensor_copy(xcb[:, c, :], xc[:, c, :]), outs=[XCB[c]], ins=[XC[c]])
                    POOL.op(lambda: G.tensor_copy(cur[:, c, 0:3], cur[:, c, 512:515]), outs=[CURS[c]], ins=[CURS[c]])
                for c in range(8):
                    ps, PS = psr.get()
                    PE.op(lambda: TE.matmul(ps[:], wa[:, c, :], xcb[:, c, :], start=True, stop=True), outs=[PS], ins=[WA, XCB[c]])
                    ACT.op(lambda: A.activation(rr[:, c, :], ps[:], AF.Sigmoid, bias=pcol(PV_BA + c)), outs=[RR[c]], ins=[PS, PVEC])
                    ps2, PS2 = psr.get()
                    PE.op(lambda: TE.matmul(ps2[:], wx[:, c, :], xcb[:, c, :], start=True, stop=True), outs=[PS2], ins=[WA, XCB[c]])
                    ACT.op(lambda: A.activation(ii[:, c, :], ps2[:], AF.Sigmoid, bias=pcol(PV_BX + c)), outs=[II[c]], ins=[PS2, PVEC])
                for c in range(8):
                    ACT.op(lambda: A.activation(aa[:, c, :], rr[:, c, :], AF.Exp, scale=cl[:, c:c + 1]), outs=[AA[c]], ins=[RR[c], CL])
                    ACT.op(lambda: A.activation(rr[:, c, :], rr[:, c, :], AF.Exp, scale=cl[:, 8 + c:9 + c]), outs=[RR[c]], ins=[RR[c], CL])
                for c in range(8):
                    ACT.op(lambda: A.activation(rr[:, c, :], rr[:, c, :], AF.Sqrt, scale=-1.0, bias=1.0), outs=[RR[c]], ins=[RR[c]])
                    POOL.op(lambda: G.tensor_tensor(ii[:, c, :], ii[:, c, :], xc[:, c, :], op=ALU.mult), outs=[II[c]], ins=[II[c], XC[c]])
                    DVE.op(lambda: V.tensor_tensor(ii[:, c, :], ii[:, c, :], rr[:, c, :], op=ALU.mult), outs=[II[c]], ins=[II[c], RR[c]])
                    DVE.op(lambda: V.tensor_tensor_scan(xc[:, c, :], aa[:, c, :], ii[:, c, :], hst[:, c:c + 1], op0=ALU.mult, op1=ALU.add), outs=[XC[c]], ins=[AA[c], II[c], HST])
                    DVE.op(lambda: V.tensor_copy(hst[:, c:c + 1], xc[:, c, 511:512]), outs=[HST], ins=[XC[c]])
                for c in range(8):
                    ps, PS = psr.get()
                    acc_mm(ps[:], PS, [(wl[:, k, 1024 + c * 128:1024 + (c + 1) * 128], hT[:, k, :]) for k in range(8)], [WL, HT])
                    gl, GL = glr.get()
                    ACT.op(lambda: A.activation(gl[:], ps[:], AF.Gelu), outs=[GL], ins=[PS])
                    POOL.op(lambda: G.tensor_tensor(mbuf[:, c, :], gl[:], xc[:, c, :], op=ALU.mult), outs=[MB], ins=[GL, XC[c]])
                POOL.dma(m_d[:, :, g * 512:(g + 1) * 512].rearrange("kc p t -> p kc t"), mbuf[:], ins=[MB])
            K.barrier()
        K.scope = K.es
        with ExitStack() as sc:
            K.scope = sc
            wg = K.sb([128, 8, D], BF16); WG = Res()
            wlo = K.sb([128, 8, D], BF16); WLO = Res()
            POOL.dma(wg[:], kc(w_in_d[:, 5120:6144]), outs=[WG])
            POOL.dma(wlo[:], kc(wlo_d), outs=[WLO])
            hTr = Rot(K, 3, [128, 8, 512], BF16)
            mr = Rot(K, 3, [128, 8, 512], BF16)
            zbr = Rot(K, 2, [128, 8, 512], BF16)
            sgr = Rot(K, 3, [128, 512], F32)
            xr = Rot(K, 4, [128, D], F32)
            psr = Rot(K, 8, [128, 512], F32, psum=True)
            for g in range(NG):
                hT, HT = hTr.get()
                SP.dma(hT[:], hT_d[:, :, g * 512:(g + 1) * 512].rearrange("kc p t -> p kc t"), outs=[HT])
                mb_, MB_ = mr.get()
                SP.dma(mb_[:], m_d[:, :, g * 512:(g + 1) * 512].rearrange("kc p t -> p kc t"), outs=[MB_])
                zbuf, ZB = zbr.get()

                def ybuild(fo, py, PY):
                    acc_mm(py[:], PY, [(wlo[:, k, fo * 128:(fo + 1) * 128], mb_[:, k, :]) for k in range(8)], [WLO, MB_])
                post_mix(g, hT, HT, ybuild, wg, WG, sgr, zbuf, ZB, psr, xr, x_d, mode=("store" if upto >= 3 else "both"))
            K.barrier()
        K.scope = K.es

    if upto >= 2:
        with ExitStack() as sc:
            K.scope = sc
            wqk = K.sb([128, 8, 2048], BF16); WQK = Res()
            wv = K.sb([128, 8, D], BF16); WV = Res()
            POOL.dma(wqk[:], kc(w_in_d[:, 2048:4096]), outs=[WQK])
            POOL.dma(wv[:], kc(w_in_d[:, 4096:5120]), outs=[WV])
            TC = min(S, 1024)
            posi = K.sb([128, TC], I32); POSI = Res()
            ang = K.sb([128, TC], F32); ANG = Res()
            nn = K.sb([128, TC], F32); NN = Res()
            cosT = K.sb([128, S], F32); COS = Res()
            sinT = K.sb([128, S], F32); SIN = Res()
            hTr = Rot(K, 2, [128, 8, 512], BF16)
            psr = Rot(K, 8, [128, 512], F32, psum=True)
            sqr = Rot(K, 4, [128, 512], BF16)
            rsr = Rot(K, 4, [128, 512], F32)
            qnr = Rot(K, 4, [128, 512], BF16)
            t1r = Rot(K, 4, [128, 512], F32)
            t2r = Rot(K, 4, [128, 512], F32)
            qor = Rot(K, 4, [128, 512], BF16)
            vtr = Rot(K, 2, [128, D], BF16)
            gq = K.sb([128, 2], F32); GQ = Res()
            DVE.op(lambda: V.tensor_scalar(gq[:, 0:1], pcol(PV_QN), 0.125, None, op0=ALU.mult), outs=[GQ], ins=[PVEC])
            DVE.op(lambda: V.tensor_copy(gq[:, 1:2], pcol(PV_KN)), outs=[GQ], ins=[PVEC])
            for b in range(nseq):
                for t0 in range(0, S, TC):
                    csl = slice(t0, t0 + TC)
                    SP.dma(posi[:], pos_d[b:b + 1, csl].partition_broadcast(128), outs=[POSI])
                    DVE.op(lambda: V.tensor_copy(ang[:], posi[:]), outs=[ANG], ins=[POSI])
                    DVE.op(lambda: V.tensor_scalar(ang[:], ang[:], cst[:, C_INVF:C_INVF + 1], None, op0=ALU.mult), outs=[ANG], ins=[ANG, CST])
                    DVE.op(lambda: V.tensor_scalar(nn[:], ang[:], 1.0 / TWO_PI, MAGIC, op0=ALU.mult, op1=ALU.add), outs=[NN], ins=[ANG])
                    DVE.op(lambda: V.tensor_scalar(nn[:], nn[:], -MAGIC, None, op0=ALU.add), outs=[NN], ins=[NN])
                    for cw in (CW1, CW2, CW3):
                        DVE.op(lambda cw=cw: V.scalar_tensor_tensor(ang[:], nn[:], -cw, ang[:], op0=ALU.mult, op1=ALU.add), outs=[ANG], ins=[NN, ANG])
                    DVE.op(lambda: V.tensor_scalar(ang[:], ang[:], math.pi, -math.pi, op0=ALU.min, op1=ALU.max), outs=[ANG], ins=[ANG])
                    ACT.op(lambda: A.activation(sinT[:, csl], ang[:], AF.Sin), outs=[SIN], ins=[ANG])
                    DVE.op(lambda: V.tensor_scalar(ang[:], ang[:], math.pi / 2, None, op0=ALU.add), outs=[ANG], ins=[ANG])
                    DVE.op(lambda: V.tensor_scalar(nn[:], ang[:], math.pi, -TWO_PI, op0=ALU.is_gt, op1=ALU.mult), outs=[NN], ins=[ANG])
                    DVE.op(lambda: V.tensor_tensor(ang[:], ang[:], nn[:], op=ALU.add), outs=[ANG], ins=[ANG, NN])
                    DVE.op(lambda: V.tensor_scalar(ang[:], ang[:], math.pi, -math.pi, op0=ALU.min, op1=ALU.max), outs=[ANG], ins=[ANG])
                    ACT.op(lambda: A.activation(cosT[:, csl], ang[:], AF.Sin), outs=[COS], ins=[ANG])
                for gi in range(GS):
                    g = b * GS + gi
                    tsl = slice(gi * 512, (gi + 1) * 512)
                    hT, HT = hTr.get()
                    SP.dma(hT[:], hT_d[:, :, g * 512:(g + 1) * 512].rearrange("kc p t -> p kc t"), outs=[HT])
                    units = [(qk, h) for qk in range(2) for h in range(8)]
                    for b0 in range(0, 16, 4):
                        bu = units[b0:b0 + 4]
                        st = []
                        for (qk, h) in bu:
                            c0 = qk * 1024 + h * 128
                            ps, PS = psr.get()
                            acc_mm(ps[:], PS, [(wqk[:, k, c0:c0 + 128], hT[:, k, :]) for k in range(8)], [WQK, HT])
                            st.append(dict(qk=qk, h=h, ps=ps, PS=PS))
                        for u in st:
                            u["sq"], u["SQ"] = sqr.get()
                            ACT.op(lambda: A.activation(u["sq"][:], u["ps"][:], AF.Square), outs=[u["SQ"]], ins=[u["PS"]])
                        for u in st:
                            u["pss"], u["PSS"] = psr.get()
                            PE.op(lambda: TE.matmul(u["pss"][:], blkb[:], u["sq"][:], start=True, stop=True), outs=[u["PSS"]], ins=[IDB, u["SQ"]])
                        for u in st:
                            u["rs"], u["RS"] = rsr.get()
                            ACT.op(lambda: A.activation(u["rs"][:], u["pss"][:], AF.Ln, scale=1.0 / 64, bias=EPS), outs=[u["RS"]], ins=[u["PSS"]])
                        for u in st:
                            ACT.op(lambda: A.activation(u["rs"][:], u["rs"][:], AF.Exp, scale=-0.5), outs=[u["RS"]], ins=[u["RS"]])
                        for u in st:
                            u["qn"], u["QN"] = qnr.get()
                            DVE.op(lambda: V.scalar_tensor_tensor(u["qn"][:], u["ps"][:], gq[:, u["qk"]:u["qk"] + 1], u["rs"][:], op0=ALU.mult, op1=ALU.mult), outs=[u["QN"]], ins=[u["PS"], GQ, u["RS"]])
                        for u in st:
                            u["pr"], u["PR"] = psr.get()
                            PE.op(lambda: TE.matmul(u["pr"][:], rotb[:], u["qn"][:], start=True, stop=True), outs=[u["PR"]], ins=[IDB, u["QN"]])
                        for u in st:
                            u["t1"], u["T1"] = t1r.get()
                            DVE.op(lambda: V.tensor_tensor(u["t1"][:], u["qn"][:], cosT[:, tsl], op=ALU.mult), outs=[u["T1"]], ins=[u["QN"], COS])
                        for u in st:
                            u["t2"], u["T2"] = t2r.get()
                            DVE.op(lambda: V.tensor_tensor(u["t2"][:], u["pr"][:], sinT[:, tsl], op=ALU.mult), outs=[u["T2"]], ins=[u["PR"], SIN])
                        for u in st:
                            qo, QO = qor.get()
                            POOL.op(lambda: G.tensor_tensor(qo[:], u["t1"][:], u["t2"][:], op=ALU.add), outs=[QO], ins=[u["T1"], u["T2"]])
                            dst = q_d if u["qk"] == 0 else k_d
                            POOL.dma(dst[u["h"], :, g * 512:(g + 1) * 512], qo[:], ins=[QO])
                    for tt in range(4):
                        t = g * 4 + tt
                        vt, VT = vtr.get()
                        for hf in range(2):
                            ps, PS = psr.get()
                            acc_mm(ps[:], PS, [(hT[:, k, tt * 128:(tt + 1) * 128], wv[:, k, hf * 512:(hf + 1) * 512]) for k in range(8)], [HT, WV])
                            ACT.op(lambda: A.copy(vt[:, hf * 512:(hf + 1) * 512], ps[:]), outs=[VT], ins=[PS])
                        POOL.dma(v_d[t * 128:(t + 1) * 128, :], vt[:], ins=[VT])
            K.barrier()
        K.scope = K.es

    if upto >= 3:
        with ExitStack() as sc:
            K.scope = sc
            NQT = S // 128
            wao = K.sb([128, 8, D], BF16); WAO = Res()
            wg = K.sb([128, 8, D], BF16); WG = Res()
            POOL.dma(wao[:], kc(wao_d), outs=[WAO])
            POOL.dma(wg[:], kc(w_in_d[:, 6144:7168]), outs=[WG])
            sub_b = K.sb([128, 128], F32); SUBB = Res()
            SP.dma(sub_b[:], gvec_d[:, GV_SUBLN:GV_SUBLN + 128], outs=[SUBB])
            DVE.op(lambda: V.tensor_scalar(sub_b[:], sub_b[:], 0.8, None, op0=ALU.mult), outs=[SUBB], ins=[SUBB])
            oT = K.sb([128, 8, S], BF16); OT = Res()
            qTr = Rot(K, 2, [128, S], BF16)
            kTr = Rot(K, 2, [128, S], BF16)
            var = Rot(K, 2, [128, NQT, 132], BF16)
            for va, VA in var.t:
                POOL.op(lambda va=va: G.memset(va[:, :, 128:129], 1.0), outs=[VA])
            pscr = [(K.ps([128, 2, 512], F32), [Res(), Res()]) for _ in range(2)]
            pscr_i = [0]

            def get_sc():
                r = pscr[pscr_i[0]]
                pscr_i[0] ^= 1
                return r
            paccb = [(K.ps([128, 512], F32), Res()) for _ in range(3)]
            ptp = (K.ps([128, 512], F32), Res())

            def acs(m, j):
                idx = m * 4 + j
                t_, R_ = paccb[idx // 3]
                return t_, R_, (idx % 3) * 132, (idx % 3 == 0)
            bankviews = _Views([(t[:, m, :], RS[m]) for (t, RS) in pscr for m in range(2)])
            Pr = Rot(K, 3, [128, 2, 512], BF16)
            smr = Rot(K, 2, [128, 8, 4], F32)
            onr = Rot(K, 1, [128, 8, 128], F32)
            o1r = Rot(K, 2, [128, 4, 128], F32)
            jkr = Rot(K, 2, [128, 4, 128], F32)
            hTr = Rot(K, 1, [128, 8, 512], BF16)
            zbuf = K.sb([128, 8, 512], BF16); ZB = Res()
            sgr = Rot(K, 1, [128, 512], F32)
            xr = Rot(K, 1, [128, D], F32)
            zlr = Rot(K, 1, [128, 512], BF16)
            deferred = [None]
            heads = {}

            def get_head(b, h):
                if (b, h) not in heads:
                    qT, QT = qTr.get()
                    kT, KT = kTr.get()
                    va, VA = var.get()
                    SP.dma(qT[:], q_d[h, :, b * S:(b + 1) * S], outs=[QT])
                    SP.dma(kT[:], k_d[h, :, b * S:(b + 1) * S], outs=[KT])
                    SP.dma(va[:, :, 0:128], v_d[b * S:(b + 1) * S, h * 128:(h + 1) * 128].rearrange("(t p) d -> p t d", p=128), outs=[VA])
                    heads[(b, h)] = (qT, QT, kT, KT, va, VA)
                return heads[(b, h)]

            def issue_scores(step):
                b, h, qg, kt = step
                qT, QT, kT, KT, va, VA = get_head(b, h)
                jmin = max(0, kt - 4 * qg)
                ncol = (4 - jmin) * 128
                q0 = qg * 512 + jmin * 128
                sp_, RS = get_sc()
                for m in range(2):
                    msl = slice(m * 64, (m + 1) * 64)
                    PE.op(lambda: TE.matmul(sp_[:, m, 0:ncol], kT[msl, kt * 128:(kt + 1) * 128], qT[msl, q0:q0 + ncol], start=True, stop=True),
                          outs=[RS[m]], ins=[KT, QT], inc=(m == 1))
                return sp_, RS, jmin, ncol

            for b in range(nseq):
                steps = [(b, h, qg, kt) for h in range(8) for qg in range(GS) for kt in range(4 * qg + 4)]
                pend = [issue_scores(steps[0]), issue_scores(steps[1])]
                for i, (b_, h, qg, kt) in enumerate(steps):
                    if qg == 0 and kt == 0 and h + 1 < 8:
                        get_head(b, h + 1)
                    qT, QT, kT, KT, va, VA = get_head(b, h)
                    nkt = 4 * qg + 4
                    sp_, RS, jmin, ncol = pend.pop(0)
                    P, PR = Pr.get()
                    ACT.op(lambda: A.activation(P[:, :, 0:ncol], sp_[:, :, 0:ncol], AF.Exp), outs=[PR], ins=RS)
                    if kt >= 4 * qg:
                        POOL.op(lambda: G.memset(P[64:128, :, 0:64], 0.0), outs=[PR], ins=[PR])
                    if i + 2 < len(steps):
                        pend.append(issue_scores(steps[i + 2]))
                    for m in range(2):
                        for j in range(jmin, 4):
                            ac, AC, col, bank_first = acs(m, j)
                            first = (kt == 0 and bank_first)
                            last = (kt == 4 * qg + j)
                            PE.op(lambda: TE.matmul(ac[:, col:col + 129], P[:, m, (j - jmin) * 128:(j - jmin + 1) * 128], va[:, kt, 0:129], start=first, stop=last),
                                  outs=[AC], ins=[PR, VA], inc=(m == 1 and j == 3))
                    if kt == 3 and deferred[0] is not None:
                        deferred[0]()
                        deferred[0] = None
                    if kt != nkt - 1:
                        continue
                    sm, SM = smr.get()
                    o1, O1 = o1r.get()
                    on, ON = onr.get()
                    jk, JK = jkr.get()
                    for bk in range(3):
                        na = 3 if bk < 2 else 2
                        t_, R_ = paccb[bk]
                        v3 = t_[:, 0:na * 132].rearrange("p (a c) -> p a c", c=132)
                        rc = sm[:, 0:2, :].rearrange("p a b -> p (a b)")[:, bk * 3:bk * 3 + na]
                        DVE.op(lambda: V.reciprocal(rc, v3[:, :, 128]), outs=[SM], ins=[R_])
                        DVE.op(lambda: V.tensor_tensor(on[:, bk * 3:bk * 3 + na, :], v3[:, :, 0:128], rc.unsqueeze(2).to_broadcast([128, na, 128]), op=ALU.mult),
                               outs=[ON], ins=[R_, SM])
                    DVE.op(lambda: V.scalar_tensor_tensor(o1[:], on[:, 4:8, :], neglam[:, 0:1], on[:, 0:4, :], op0=ALU.mult, op1=ALU.add), outs=[O1], ins=[ON, NEGLAM])
                    DVE.op(lambda: V.tensor_tensor(jk[:], o1[:], o1[:], op=ALU.mult), outs=[JK], ins=[O1])
                    DVE.op(lambda: V.reduce_sum(sm[:, 3, :], jk[:], axis=AX.X), outs=[SM], ins=[JK])

                    def part_b(sm=sm, SM=SM, o1=o1, O1=O1, jk=jk, JK=JK, h=h, qg=qg):
                        ACT.op(lambda: A.activation(sm[:, 4, :], sm[:, 3, :], AF.Ln, scale=1.0 / 128, bias=EPS), outs=[SM], ins=[SM])
                        ACT.op(lambda: A.activation(sm[:, 5, :], sm[:, 4, :], AF.Exp, scale=-0.5), outs=[SM], ins=[SM])
                        for j in range(4):
                            DVE.op(lambda: V.scalar_tensor_tensor(jk[:, j, :], o1[:, j, :], sm[:, 5, j:j + 1], sub_b[:], op0=ALU.mult, op1=ALU.mult), outs=[JK], ins=[O1, SM, SUBB])
                        tp_, TP = ptp
                        for j in range(4):
                            PE.op(lambda: TE.transpose(tp_[:, j * 128:(j + 1) * 128], jk[:, j, :], identf), outs=[TP], ins=[JK, CST], inc=(j == 3))
                        DVE.op(lambda: V.tensor_copy(oT[:, h, qg * 512:(qg + 1) * 512], tp_[:, :]), outs=[OT], ins=[TP])
                    deferred[0] = part_b
                if deferred[0] is not None:
                    deferred[0]()
                    deferred[0] = None
                for gi in range(GS):
                    g = b * GS + gi
                    hT, HT = hTr.get()
                    SP.dma(hT[:], hT_d[:, :, g * 512:(g + 1) * 512].rearrange("kc p t -> p kc t"), outs=[HT])
                    def ybuild(fo, py, PY):
                        acc_mm(py[:], PY, [(wao[:, hh_, fo * 128:(fo + 1) * 128], oT[:, hh_, gi * 512:(gi + 1) * 512]) for hh_ in range(8)], [WAO, OT])
                    post_mix(g, hT, HT, ybuild, wg, WG, sgr, zbuf, ZB, bankviews, xr, x_d, mode="add", zlr=zlr)
            K.barrier()
        K.scope = K.es

    if upto >= 4:
        with ExitStack() as sc:
            K.scope = sc
            wcq = K.sb([128, 8, 512], BF16); WCQ = Res()
            wckv = K.sb([128, 8, D], BF16); WCKV = Res()
            wco = K.sb([128, 4, D], BF16); WCO = Res()
            POOL.dma(wcq[:], kc(wcq_d), outs=[WCQ])
            POOL.dma(wckv[:], kc(wckv_d), outs=[WCKV])
            POOL.dma(wco[:], kc(wco_d), outs=[WCO])
            gcx = K.sb([128, D], F32); GCX = Res()
            gmem = K.sb([128, D], F32); GMEM = Res()
            SP.dma(gcx[:], gvec_d[:, GV_CX:GV_CX + D], outs=[GCX])
            SP.dma(gmem[:], gvec_d[:, GV_MEM:GV_MEM + D], outs=[GMEM])
            gq = K.sb([128, 1], F32); GQ = Res()
            DVE.op(lambda: V.tensor_scalar(gq[:], pcol(PV_CQN), 128.0 ** -0.5, None, op0=ALU.mult), outs=[GQ], ins=[PVEC])
            xr = Rot(K, 5, [128, D], F32)
            hbr = Rot(K, 2, [128, D], BF16)
            junk = K.sb([128, D], F32); JK = Res()
            str_ = Rot(K, 4, [128, 4], F32)
            ptr = Rot(K, 2, [128, 8, 128], BF16, psum=True)
            psr = Rot(K, 6, [128, 512], F32, psum=True)
            memT = K.sb([128, 8, 256], BF16); MEMT = Res()
            kcT = K.sb([128, 4, 256], BF16); KCT = Res()
            vc = K.sb([128, 2, 512], BF16); VC = Res()
            hcT = K.sb([128, 8, 512], BF16); HCT = Res()
            sqr = Rot(K, 4, [128, 512], BF16)
            rsr = Rot(K, 8, [128, 512], F32)
            qnr = Rot(K, 4, [128, 512], BF16)
            qfr = Rot(K, 4, [128, 512], F32)
            Pr = Rot(K, 4, [128, 2, 512], BF16)
            ocT = K.sb([128, 4, 512], BF16); OCT = Res()
            for b in range(nseq):
                for mt in range(2):
                    xt, XT = xr.get()
                    SP.dma(xt[:], mem_d[b * 256 + mt * 128:b * 256 + (mt + 1) * 128, :], outs=[XT])
                    hb, HB = hbr.get()
                    st, ST = str_.get()
                    rmsnorm_tile(xt[:], XT, gmem[:], GMEM, hb[:], HB, junk, JK, st, ST)
                    pt, PT = ptr.get()
                    transpose8(hb, HB, pt, PT, memT[:, :, mt * 128:(mt + 1) * 128], MEMT, DVE)
                for h in range(4):
                    ps, PS = psr.get()
                    acc_mm(ps[:, 0:256], PS, [(wckv[:, k, h * 128:(h + 1) * 128], memT[:, k, :]) for k in range(8)], [WCKV, MEMT])
                    sq, SQ = sqr.get()
                    ACT.op(lambda: A.activation(sq[:, 0:256], ps[:, 0:256], AF.Square), outs=[SQ], ins=[PS])
                    pss, PSS = psr.get()
                    PE.op(lambda: TE.matmul(pss[:, 0:256], onesb[:], sq[:, 0:256], start=True, stop=True), outs=[PSS], ins=[IDB, SQ])
                    rs, RS = rsr.get()
                    ACT.op(lambda: A.activation(rs[:, 0:256], pss[:, 0:256], AF.Ln, scale=1.0 / 128, bias=EPS), outs=[RS], ins=[PSS])
                    ACT.op(lambda: A.activation(rs[:, 0:256], rs[:, 0:256], AF.Exp, scale=-0.5), outs=[RS], ins=[RS])
                    DVE.op(lambda: V.scalar_tensor_tensor(kcT[:, h, :], ps[:, 0:256], pcol(PV_CKN), rs[:, 0:256], op0=ALU.mult, op1=ALU.mult), outs=[KCT], ins=[PS, PVEC, RS])
                for mt in range(2):
                    ps, PS = psr.get()
                    acc_mm(ps[:], PS, [(memT[:, k, mt * 128:(mt + 1) * 128], wckv[:, k, 512:1024]) for k in range(8)], [MEMT, WCKV])
                    ACT.op(lambda: A.copy(vc[:, mt, :], ps[:]), outs=[VC], ins=[PS])
                for gi in range(GS):
                    g = b * GS + gi
                    xts = []
                    for tt in range(4):
                        t = g * 4 + tt
                        xt, XT = xr.get()
                        xts.append((xt, XT))
                        SP.dma(xt[:], out_d[t * 128:(t + 1) * 128, :], outs=[XT])
                        hb, HB = hbr.get()
                        st, ST = str_.get()
                        rmsnorm_tile(xt[:], XT, gcx[:], GCX, hb[:], HB, junk, JK, st, ST)
                        pt, PT = ptr.get()
                        transpose8(hb, HB, pt, PT, hcT[:, :, tt * 128:(tt + 1) * 128], HCT, ACT if tt % 2 else DVE)
                    hs = [dict(h=h) for h in range(4)]
                    for u in hs:
                        h = u["h"]
                        ps, PS = psr.get()
                        acc_mm(ps[:], PS, [(wcq[:, k, h * 128:(h + 1) * 128], hcT[:, k, :]) for k in range(8)], [WCQ, HCT])
                        u["qf"], u["QF"] = qfr.get()
                        ACT.op(lambda: A.copy(u["qf"][:], ps[:]), outs=[u["QF"]], ins=[PS])
                    for u in hs:
                        u["sq"], u["SQ"] = sqr.get()
                        ACT.op(lambda: A.activation(u["sq"][:], u["qf"][:], AF.Square), outs=[u["SQ"]], ins=[u["QF"]])
                    for u in hs:
                        u["pss"], u["PSS"] = psr.get()
                        PE.op(lambda: TE.matmul(u["pss"][:], onesb[:], u["sq"][:], start=True, stop=True), outs=[u["PSS"]], ins=[IDB, u["SQ"]])
                    for u in hs:
                        u["rs"], u["RS"] = rsr.get()
                        ACT.op(lambda: A.activation(u["rs"][:], u["pss"][:], AF.Ln, scale=1.0 / 128, bias=EPS), outs=[u["RS"]], ins=[u["PSS"]])
                    for u in hs:
                        ACT.op(lambda: A.activation(u["rs"][:], u["rs"][:], AF.Exp, scale=-0.5), outs=[u["RS"]], ins=[u["RS"]])
                    for u in hs:
                        u["qn"], u["QN"] = qnr.get()
                        DVE.op(lambda: V.scalar_tensor_tensor(u["qn"][:], u["qf"][:], gq[:, 0:1], u["rs"][:], op0=ALU.mult, op1=ALU.mult), outs=[u["QN"]], ins=[u["QF"], GQ, u["RS"]])
                    for u in hs:
                        h = u["h"]
                        u["P"], u["PRr"] = Pr.get()
                        for mt in range(2):
                            psc, PSC = psr.get()
                            PE.op(lambda: TE.matmul(psc[:], kcT[:, h, mt * 128:(mt + 1) * 128], u["qn"][:], start=True, stop=True), outs=[PSC], ins=[KCT, u["QN"]])
                            ACT.op(lambda: A.activation(u["P"][:, mt, :], psc[:], AF.Exp), outs=[u["PRr"]], ins=[PSC])
                    for u in hs:
                        h = u["h"]
                        po, PO = psr.get()
                        acc_mm(po[:], PO, [(vc[:, mt, h * 128:(h + 1) * 128], u["P"][:, mt, :]) for mt in range(2)], [VC, u["PRr"]])
                        psm, PSM = psr.get()
                        acc_mm(psm[:], PSM, [(onesb[:], u["P"][:, mt, :]) for mt in range(2)], [IDB, u["PRr"]])
                        rs2, RS2 = rsr.get()
                        DVE.op(lambda: V.reciprocal(rs2[:], psm[:]), outs=[RS2], ins=[PSM])
                        DVE.op(lambda: V.tensor_tensor(ocT[:, h, :], po[:], rs2[:], op=ALU.mult), outs=[OCT], ins=[PO, RS2])
                    for tt in range(4):
                        t = g * 4 + tt
                        xt, XT = xts[tt]
                        for hf in range(2):
                            po, PO = psr.get()
                            acc_mm(po[:], PO, [(ocT[:, hh_, tt * 128:(tt + 1) * 128], wco[:, hh_, hf * 512:(hf + 1) * 512]) for hh_ in range(4)], [OCT, WCO])
                            DVE.op(lambda: V.tensor_tensor(xt[:, hf * 512:(hf + 1) * 512], xt[:, hf * 512:(hf + 1) * 512], po[:], op=ALU.add), outs=[XT], ins=[XT, PO])
                        POOL.dma(out_d[t * 128:(t + 1) * 128, :], xt[:], ins=[XT])
            K.barrier()
        K.scope = K.es

    if upto >= 5:
        with ExitStack() as sc:
            K.scope = sc
            slots = K.sb([128, NT, 2], I32); SL = Res()
            wts = K.sb([128, NT, 2], F32); WT = Res()
            with ExitStack() as sc1:
                K.scope = sc1
                gff = K.sb([128, D], F32); GFF = Res()
                brg = K.sb([128, 36], F32); BRG = Res()
                wrg = K.sb([128, 8, 36], F32); WRG = Res()
                SP.dma(gff[:], gvec_d[:, GV_FFN:GV_FFN + D], outs=[GFF])
                SP.dma(brg[:], gvec_d[:, GV_BRG:GV_BRG + 36], outs=[BRG])
                SP.dma(wrg[:], kc(wrg_d), outs=[WRG])
                base = K.sb([128, 32], F32); BASE = Res()
                DVE.op(lambda: V.tensor_copy(base[:], cst[:, C_EOFF:C_EOFF + 32]), outs=[BASE], ins=[CST])
                xr = Rot(K, 3, [128, D], F32)
                hfr = Rot(K, 2, [128, D], F32)
                hbr = Rot(K, 10, [128, D], BF16)
                junk = K.sb([128, D], F32); JK = Res()
                str_ = Rot(K, 4, [128, 4], F32)
                ptf = Rot(K, 2, [128, 4, 128], F32, psum=True)
                hTf = Rot(K, 2, [128, 8, 128], F32)
                pl = Rot(K, 3, [128, 64], F32, psum=True)
                prk = Rot(K, 2, [128, 5, 32], F32, psum=True)
                lgr = Rot(K, 2, [128, 4, 36], F32)
                w1r = Rot(K, 2, [128, 16, 4], F32)
                ohgr = Rot(K, 2, [128, 4, 4], F32)
                d4r = Rot(K, 2, [128, 4, 4], F32)
                elr = Rot(K, 2, [128, 3, 4, 8], F32)
                mkr = Rot(K, 2, [128, 2, 4, 8], F32)
                prr = Rot(K, 2, [128, 4, 32], F32)
                ohr = Rot(K, 2, [128, 3, 4, 32], F32)
                ohb = Rot(K, 2, [128, 4, 32], BF16)
                rkr = Rot(K, 2, [128, 4, 32], F32)
                trib = K.sb([128, 128], BF16); TRIB = Res()
                DVE.op(lambda: V.tensor_copy(trib[:], cst[:, C_TRI:C_TRI + 128]), outs=[TRIB], ins=[CST])
                bc = lambda ap, shape, ax: ap.unsqueeze(ax).to_broadcast(shape)
                for tb in range(NT // 4):
                    lg, LG = lgr.get()
                    hbs = []
                    for ti in range(4):
                        t = tb * 4 + ti
                        xt, XT = xr.get()
                        SP.dma(xt[:], out_d[t * 128:(t + 1) * 128, :], outs=[XT])
                        hf, HF = hfr.get()
                        st, ST = str_.get()
                        rmsnorm_tile(xt[:], XT, gff[:], GFF, hf[:], HF, junk, JK, st, ST)
                        hb, HB = hbr.get()
                        hbs.append((hb, HB))
                        ACT.op(lambda: A.copy(hb[:], hf[:]), outs=[HB], ins=[HF])
                        hT, HT = hTf.get()
                        for half in range(2):
                            pt, PT = ptf.get()
                            for c in range(4):
                                cc = half * 4 + c
                                PE.op(lambda: TE.transpose(pt[:, c, :], hf[:, cc * 128:(cc + 1) * 128], identf), outs=[PT], ins=[HF, CST], inc=(c == 3))
                            if half:
                                ACT.op(lambda: A.copy(hT[:, 4:8, :], pt[:]), outs=[HT], ins=[PT])
                            else:
                                DVE.op(lambda: V.tensor_copy(hT[:, 0:4, :], pt[:]), outs=[HT], ins=[PT])
                        lp, LP = pl.get()
                        acc_mm(lp[:, 0:36], LP, [(hT[:, k, :], wrg[:, k, :]) for k in range(8)], [HT, WRG])
                        DVE.op(lambda: V.tensor_tensor(lg[:, ti, :], lp[:, 0:36], brg[:], op=ALU.add), outs=[LG], ins=[LP, BRG])
                    w1, W1 = w1r.get()
                    ohg, OHG = ohgr.get()
                    d4, D4 = d4r.get()
                    el, EL = elr.get()
                    mk, MK = mkr.get()
                    pr_, PRR = prr.get()
                    oh, OH = ohr.get()
                    lgg = lg[:, :, 0:4]
                    S4 = [128, 4, 4]
                    S8 = [128, 4, 8]
                    DVE.op(lambda: V.reduce_max(w1[:, 0, :], lgg, axis=AX.X), outs=[W1], ins=[LG])
                    DVE.op(lambda: V.tensor_tensor(ohg[:], lgg, bc(w1[:, 0, :], S4, 2), op=ALU.is_equal), outs=[OHG], ins=[LG, W1])
                    DVE.op(lambda: V.tensor_tensor(d4[:], lgg, bc(w1[:, 0, :], S4, 2), op=ALU.subtract), outs=[D4], ins=[LG, W1])
                    ACT.op(lambda: A.activation(d4[:], d4[:], AF.Exp), outs=[D4], ins=[D4])
                    DVE.op(lambda: V.reduce_sum(w1[:, 1, :], d4[:], axis=AX.X), outs=[W1], ins=[D4])
                    DVE.op(lambda: V.reciprocal(w1[:, 2, :], w1[:, 1, :]), outs=[W1], ins=[W1])
                    lge = lg[:, :, 4:36].rearrange("p t (g e) -> p t g e", g=4)
                    p4 = pr_[:].rearrange("p t (g e) -> p t g e", g=4)
                    DVE.op(lambda: V.tensor_tensor(p4, lge, bc(ohg[:], [128, 4, 4, 8], 3), op=ALU.mult), outs=[PRR], ins=[LG, OHG])
                    DVE.op(lambda: V.reduce_sum(el[:, 0, :, :], pr_[:].rearrange("p t (g e) -> p t e g", g=4), axis=AX.X), outs=[EL], ins=[PRR])
                    DVE.op(lambda: V.reduce_max(w1[:, 3, :], el[:, 0, :, :], axis=AX.X), outs=[W1], ins=[EL])
                    DVE.op(lambda: V.tensor_tensor(mk[:, 0, :, :], el[:, 0, :, :], bc(w1[:, 3, :], S8, 2), op=ALU.is_equal), outs=[MK], ins=[EL, W1])
                    DVE.op(lambda: V.scalar_tensor_tensor(el[:, 1, :, :], mk[:, 0, :, :], -1e30, el[:, 0, :, :], op0=ALU.mult, op1=ALU.add), outs=[EL], ins=[MK, EL])
                    DVE.op(lambda: V.reduce_max(w1[:, 4, :], el[:, 1, :, :], axis=AX.X), outs=[W1], ins=[EL])
                    DVE.op(lambda: V.tensor_tensor(mk[:, 1, :, :], el[:, 1, :, :], bc(w1[:, 4, :], S8, 2), op=ALU.is_equal), outs=[MK], ins=[EL, W1])
                    DVE.op(lambda: V.tensor_tensor(w1[:, 5, :], w1[:, 4, :], w1[:, 3, :], op=ALU.subtract), outs=[W1], ins=[W1])
                    ACT.op(lambda: A.activation(w1[:, 6, :], w1[:, 5, :], AF.Exp), outs=[W1], ins=[W1])
                    DVE.op(lambda: V.tensor_scalar(w1[:, 6, :], w1[:, 6, :], 1.0, None, op0=ALU.add), outs=[W1], ins=[W1])
                    DVE.op(lambda: V.reciprocal(w1[:, 7, :], w1[:, 6, :]), outs=[W1], ins=[W1])
                    wt4 = wts[:, tb * 4:(tb + 1) * 4, :]
                    DVE.op(lambda: V.tensor_tensor(wt4[:, :, 0], w1[:, 7, :], w1[:, 2, :], op=ALU.mult), outs=[WT], ins=[W1])
                    DVE.op(lambda: V.tensor_tensor(wt4[:, :, 1], w1[:, 2, :], wt4[:, :, 0], op=ALU.subtract), outs=[WT], ins=[W1, WT])
                    for kk in range(2):
                        o4 = oh[:, kk, :, :].rearrange("p t (g e) -> p t g e", g=4)
                        DVE.op(lambda: V.tensor_tensor(o4, bc(ohg[:], [128, 4, 4, 8], 3), bc(mk[:, kk, :, :], [128, 4, 4, 8], 2), op=ALU.mult), outs=[OH], ins=[OHG, MK])
                    DVE.op(lambda: V.tensor_tensor(oh[:, 2, :, :], oh[:, 0, :, :], oh[:, 1, :, :], op=ALU.add), outs=[OH], ins=[OH])
                    o12, O12 = ohb.get()
                    POOL.op(lambda: G.tensor_copy(o12[:], oh[:, 2, :, :]), outs=[O12], ins=[OH])
                    rp, RP = prk.get()
                    mms = []
                    for ti in range(4):
                        mms.append((rp[:, ti, :], trib[:], o12[:, ti, :]))
                        for tj in range(ti):
                            mms.append((rp[:, ti, :], onesb[:], o12[:, tj, :]))
                    for tj in range(4):
                        mms.append((rp[:, 4, :], onesb[:], o12[:, tj, :]))
                    for i, (o_, l_, r_) in enumerate(mms):
                        PE.op(lambda: TE.matmul(o_, l_, r_, start=(i == 0), stop=(i == len(mms) - 1)), outs=[RP], ins=[TRIB, IDB, O12], inc=(i == len(mms) - 1))
                    rk, RK = rkr.get()
                    DVE.op(lambda: V.tensor_tensor(rk[:], rp[:, 0:4, :], bc(base[:], [128, 4, 32], 1), op=ALU.add), outs=[RK], ins=[RP, BASE])
                    DVE.op(lambda: V.tensor_tensor(base[:], base[:], rp[:, 4, :], op=ALU.add), outs=[BASE], ins=[BASE, RP])
                    for kk in range(2):
                        DVE.op(lambda: V.tensor_tensor(pr_[:], rk[:], oh[:, kk, :, :], op=ALU.mult), outs=[PRR], ins=[RK, OH])
                        DVE.op(lambda: V.reduce_sum(w1[:, 8 + kk, :], pr_[:], axis=AX.X), outs=[W1], ins=[PRR])
                    DVE.op(lambda: V.tensor_scalar(w1[:, 8:10, :], w1[:, 8:10, :], float(NSLOT - 1), None, op0=ALU.min), outs=[W1], ins=[W1])
                    sl4 = slots[:, tb * 4:(tb + 1) * 4, :]
                    DVE.op(lambda: V.tensor_copy(sl4.rearrange("p t k -> p k t"), w1[:, 8:10, :]), outs=[SL], ins=[W1])
                    for ti in range(4):
                        t = tb * 4 + ti
                        hb, HB = hbs[ti]
                        for kk in range(2):
                            POOL.dma(None, None, ins=[HB, SL], fn=lambda: G.indirect_dma_start(
                                out=xs_d, out_offset=bass.IndirectOffsetOnAxis(ap=slots[:, t, kk:kk + 1], axis=0), in_=hb[:], in_offset=None))
                SP.dma(cnt_d, base[:], ins=[BASE])
                K.barrier()
            K.scope = sc
            with ExitStack() as sc2:
                K.scope = sc2
                wgur = Rot(K, 2, [128, 8, D], BF16)
                wdr = Rot(K, 2, [128, 4, D], BF16)
                xsr = Rot(K, 3, [128, D], BF16)
                ptr = Rot(K, 2, [128, 8, 128], BF16, psum=True)
                psr = Rot(K, 5, [128, 512], F32, psum=True)
                xsT = Rot(K, 2, [128, 8, 512], BF16)
                slr = Rot(K, 2, [128, 512], F32)
                actT = Rot(K, 2, [128, 4, 512], BF16)
                yr = Rot(K, 3, [128, D], BF16)
                chunks = []
                c0 = 0
                while c0 < cap:
                    n = min(512, cap - c0)
                    chunks.append((c0, n))
                    c0 += n
                def load_w(e):
                    wgu, WGU = wgur.get()
                    wd, WD = wdr.get()
                    POOL.dma(wgu[:], kc(wgu_d[e]), outs=[WGU])
                    POOL.dma(wd[:], kc(wd_d[e]), outs=[WD])
                    return wgu, WGU, wd, WD
                items = [(e, c0, n) for e in range(32) for (c0, n) in chunks]

                def stage_a(e, c0, n):
                    xT, XT_ = xsT.get()
                    for j in range(n // 128):
                        r0 = e * cap + c0 + j * 128
                        xs, XS = xsr.get()
                        SP.dma(xs[:], xs_d[r0:r0 + 128, :], outs=[XS])
                        pt, PT = ptr.get()
                        transpose8(xs, XS, pt, PT, xT[:, :, j * 128:(j + 1) * 128], XT_, ACT if j % 2 else DVE)
                    return xT, XT_

                def stage_b(xT, XT_, wgu, WGU, n):
                    aT, AT = actT.get()
                    for f in range(4):
                        pg, PG = psr.get()
                        acc_mm(pg[:, 0:n], PG, [(wgu[:, k, f * 128:(f + 1) * 128], xT[:, k, 0:n]) for k in range(8)], [WGU, XT_])
                        pu, PU = psr.get()
                        acc_mm(pu[:, 0:n], PU, [(wgu[:, k, 512 + f * 128:512 + (f + 1) * 128], xT[:, k, 0:n]) for k in range(8)], [WGU, XT_])
                        sl_, SL_ = slr.get()
                        ACT.op(lambda: A.activation(sl_[:, 0:n], pg[:, 0:n], AF.Silu), outs=[SL_], ins=[PG])
                        DVE.op(lambda: V.tensor_tensor(aT[:, f, 0:n], sl_[:, 0:n], pu[:, 0:n], op=ALU.mult), outs=[AT], ins=[SL_, PU])
                    return aT, AT

                def stage_c(aT, AT, wd, WD, e, c0, n):
                    for j in range(n // 128):
                        r0 = e * cap + c0 + j * 128
                        y, Y = yr.get()
                        for hf in range(2):
                            po, PO = psr.get()
                            acc_mm(po[:], PO, [(aT[:, f, j * 128:(j + 1) * 128], wd[:, f, hf * 512:(hf + 1) * 512]) for f in range(4)], [AT, WD])
                            if hf:
                                ACT.op(lambda: A.copy(y[:, 512:1024], po[:]), outs=[Y], ins=[PO])
                            else:
                                DVE.op(lambda: V.tensor_copy(y[:, 0:512], po[:]), outs=[Y], ins=[PO])
                        SP.dma(ys_d[r0:r0 + 128, :], y[:], ins=[Y])

                wts_e = {0: load_w(0)}
                xa = stage_a(*items[0])
                for i, (e, c0, n) in enumerate(items):
                    if c0 == 0 and e + 1 < 32:
                        wts_e[e + 1] = load_w(e + 1)
                        wts_e.pop(e - 1, None)
                    wgu, WGU, wd, WD = wts_e[e]
                    xT, XT_ = xa
                    aT, AT = stage_b(xT, XT_, wgu, WGU, n)
                    if i + 1 < len(items):
                        xa = stage_a(*items[i + 1])
                    stage_c(aT, AT, wd, WD, e, c0, n)
                K.barrier()
            K.scope = sc
            with ExitStack() as sc3:
                K.scope = sc3
                xr = Rot(K, 4, [128, D], F32)
                y1r = Rot(K, 4, [128, D], BF16)
                y2r = Rot(K, 4, [128, D], BF16)

                def m3_load(t):
                    xt, XT = xr.get()
                    SP.dma(xt[:], out_d[t * 128:(t + 1) * 128, :], outs=[XT])
                    ys = []
                    for kk, rot in enumerate((y1r, y2r)):
                        y, Y = rot.get()
                        ys.append((y, Y))
                        POOL.dma(None, None, outs=[Y], ins=[SL], fn=lambda kk=kk, y=y: G.indirect_dma_start(
                            out=y[:], out_offset=None, in_=ys_d, in_offset=bass.IndirectOffsetOnAxis(ap=slots[:, t, kk:kk + 1], axis=0)))
                    return xt, XT, ys
                PF = 2
                loaded = [m3_load(t) for t in range(min(PF, NT))]
                for t in range(NT):
                    if t + PF < NT:
                        loaded.append(m3_load(t + PF))
                    xt, XT, ys = loaded.pop(0)
                    for kk in range(2):
                        y, Y = ys[kk]
                        DVE.op(lambda kk=kk, y=y: V.scalar_tensor_tensor(xt[:], y[:], wts[:, t, kk:kk + 1], xt[:], op0=ALU.mult, op1=ALU.add), outs=[XT], ins=[Y, WT, XT])
                    ACT.dma(out_d[t * 128:(t + 1) * 128, :], xt[:], ins=[XT])
                K.barrier()
        K.scope = K.es
    K.barrier()
    K.es.close()
    return nc


class _Views:
    def __init__(self, items):
        self.items = items
        self.i = 0

    def get(self):
        r = self.items[self.i]
        self.i = (self.i + 1) % len(self.items)
        return r


def _consts(cap):
    c = np.zeros((128, C_N), np.float32)
    c[:, C_IDF:C_IDF + 128] = np.eye(128, dtype=np.float32)
    c[:, C_ONES:C_ONES + 128] = 1.0
    for m in range(2):
        c[m * 64:(m + 1) * 64, C_BLK + m * 64:C_BLK + (m + 1) * 64] = 1.0
    for m in range(2):
        for d in range(8):
            c[m * 64 + d + 8, C_ROT + m * 64 + d] = -1.0
            c[m * 64 + d, C_ROT + m * 64 + d + 8] = 1.0
    c[:, C_TRI:C_TRI + 128] = np.triu(np.ones((128, 128), np.float32), k=1)
    inv_freq = np.exp(-math.log(THETA) * np.arange(HALF, dtype=np.float32) / HALF).astype(np.float32)
    for p in range(128):
        d = p % 64
        c[p, C_INVF] = inv_freq[d % 8] if d < ROT_DIMS else 0.0
    c[:, C_EOFF:C_EOFF + 32] = (np.arange(32, dtype=np.float32) * cap)[None, :]
    return c


def _pack(inp, cap):
    f = lambda a: np.ascontiguousarray(np.asarray(a, dtype=np.float32))
    pv = np.zeros((128, PV_N), np.float32)
    cw = f(inp["conv_w"])[0]
    pv[:, PV_CONVW:PV_CONVW + 32] = cw.reshape(4, 8, 128).transpose(2, 1, 0).reshape(128, 32)
    for off, name in ((PV_CONVB, "conv_b"), (PV_BA, "lru_ba"), (PV_BX, "lru_bx"), (PV_LAM, "lru_lambda")):
        pv[:, off:off + 8] = f(inp[name])[0].reshape(8, 128).T
    pv[:, PV_QN] = np.tile(f(inp["q_norm"])[0], 2)
    pv[:, PV_KN] = np.tile(f(inp["k_norm"])[0], 2)
    pv[:, PV_CQN] = f(inp["cq_norm"])[0]
    pv[:, PV_CKN] = f(inp["ck_norm"])[0]
    gv = np.zeros((GV_N,), np.float32)
    gv[GV_MIX:GV_MIX + D] = f(inp["norm_mix"])[0]
    gv[GV_CX:GV_CX + D] = f(inp["norm_cx"])[0]
    gv[GV_MEM:GV_MEM + D] = f(inp["norm_mem"])[0]
    gv[GV_FFN:GV_FFN + D] = f(inp["norm_ffn"])[0]
    gv[GV_SUBLN:GV_SUBLN + 128] = f(inp["subln"])[0]
    gv[GV_LAMV:GV_LAMV + 256] = np.concatenate([f(inp[n])[0] for n in ("lambda_q1", "lambda_k1", "lambda_q2", "lambda_k2")])
    gv[GV_BRG:GV_BRG + 36] = np.concatenate([f(inp["b_group"])[0], f(inp["b_router"])[0]])
    gvb = np.ascontiguousarray(np.broadcast_to(gv[None, :], (128, GV_N)))
    shared = {
        "w_in": f(inp["w_in"])[0], "pvec": pv, "gvec": gvb, "cst": _consts(cap),
        "lru_wa": f(inp["lru_wa"])[0], "lru_wx": f(inp["lru_wx"])[0],
        "w_lru_o": f(inp["w_lru_o"])[0], "w_attn_o": f(inp["w_attn_o"])[0], "w_out": f(inp["w_out"])[0],
        "w_cq": f(inp["w_cq"])[0], "w_ckv": f(inp["w_ckv"])[0], "w_co": f(inp["w_co"])[0],
        "w_rg": np.ascontiguousarray(np.concatenate([f(inp["w_group"])[0], f(inp["w_router"])[0]], axis=1)),
        "w_gate_up": f(inp["w_gate_up"])[0], "w_down": f(inp["w_down"])[0],
    }
    return shared


def run(inp, n_cores, nseq, S, cap, upto=99):
    nc = build(nseq, S, cap, upto)
    shared = _pack(inp, cap)
    x = np.asarray(inp["x"], np.float32)
    mem = np.asarray(inp["mem"], np.float32)
    pos = np.asarray(inp["positions"], np.int32)
    in_maps = []
    for c in range(n_cores):
        b0 = c * nseq
        m = dict(shared)
        m["x"] = np.ascontiguousarray(x[b0:b0 + nseq, :S].reshape(nseq * S, D))
        m["mem"] = np.ascontiguousarray(mem[b0:b0 + nseq].reshape(nseq * 256, D))
        m["pos"] = np.ascontiguousarray(pos[b0:b0 + nseq, :S])
        in_maps.append(m)
    res = run_bass_kernel_spmd(nc, in_maps, core_ids=list(range(n_cores)))
    outs = [np.asarray(r["out"]).reshape(nseq, S, D) for r in res.results]
    global LAST_CNT
    LAST_CNT = [np.asarray(r["cnt"])[0] for r in res.results] if "cnt" in res.results[0] else None
    return np.concatenate(outs, axis=0)


def kernel(**inputs):
    return run(inputs, N_CORES, 2, 4096, 1024).astype(np.float32)
```

```python
import math
import numpy as np
from contextlib import ExitStack
import concourse.bass as bass
import concourse.mybir as mybir
from concourse.bass_utils import run_bass_kernel_spmd

F32 = mybir.dt.float32
BF16 = mybir.dt.bfloat16
I32 = mybir.dt.int32
ALU = mybir.AluOpType
AF = mybir.ActivationFunctionType
AX = mybir.AxisListType

D = 1024
EPS = 1e-6
N_CORES = 8


class Res:
    __slots__ = ("w", "r")

    def __init__(self):
        self.w = None
        self.r = {}


class Ring:
    def __init__(self, K, n, name):
        self.sems = [K.new_sem(f"{name}{i}") for i in range(n)]
        self.cnt = [0] * n
        self.i = 0

    def next(self):
        i = self.i
        self.i = (i + 1) % len(self.sems)
        return i


class Q:
    def __init__(self, K, eng, name, is_pe=False):
        self.eng = eng
        self.sem = K.new_sem("q_" + name)
        self.n = 0
        self.seen = {}
        self.is_pe = is_pe
        self.ring = None

    def need(self, tok):
        if tok is None:
            return
        sem, val = tok
        if self.is_pe and sem is self.sem:
            return
        k = id(sem)
        if self.seen.get(k, 0) >= val:
            return
        self.eng.wait_ge(sem, val)
        self.seen[k] = val

    def _deps(self, outs, ins):
        for t in ins:
            self.need(t.w)
        for t in outs:
            self.need(t.w)
            for tok in t.r.values():
                self.need(tok)

    def _mark(self, tok, outs, ins):
        k = id(tok[0])
        for t in ins:
            t.r[k] = tok
        for t in outs:
            t.w = tok
            t.r = {}

    def op(self, fn, outs=(), ins=(), inc=True):
        self._deps(outs, ins)
        inst = fn()
        if inc:
            self.n += 1
            inst.then_inc(self.sem, 1)
            tok = (self.sem, self.n)
        else:
            tok = (self.sem, self.n + 1)
        self._mark(tok, outs, ins)
        return tok

    def dma(self, out_ap, in_ap, outs=(), ins=(), fn=None):
        ring = self.ring
        i = ring.next()
        sem = ring.sems[i]
        self.need((sem, ring.cnt[i] * 16))
        self._deps(outs, ins)
        inst = self.eng.dma_start(out=out_ap, in_=in_ap) if fn is None else fn()
        inst.then_inc(sem, 16)
        ring.cnt[i] += 1
        tok = (sem, ring.cnt[i] * 16)
        self._mark(tok, outs, ins)
        return tok


class Kern:
    def __init__(self):
        self.nc = bass.Bass("TRN2", target_bir_lowering=False)
        self.es = ExitStack()
        nc = self.nc
        self.PE = Q(self, nc.tensor, "pe", is_pe=True)
        self.ACT = Q(self, nc.scalar, "act")
        self.DVE = Q(self, nc.vector, "dve")
        self.POOL = Q(self, nc.gpsimd, "pool")
        self.SP = Q(self, nc.sync, "sp")
        self.SP.ring = Ring(self, 30, "rsp")
        self.POOL.ring = Ring(self, 30, "rpl")
        self.ACT.ring = Ring(self, 8, "rac")
        self.engs = [self.PE, self.ACT, self.DVE, self.POOL, self.SP]
        self._n = 0
        self.scope = self.es

    def new_sem(self, name):
        return self.es.enter_context(self.nc.semaphore(name))

    def sb(self, shape, dt):
        self._n += 1
        return self.scope.enter_context(self.nc.sbuf_tensor(f"t{self._n}", list(shape), dt))

    def ps(self, shape, dt):
        self._n += 1
        return self.scope.enter_context(self.nc.psum_tensor(f"p{self._n}", list(shape), dt))

    def barrier(self):
        for e in self.engs:
            for e2 in self.engs:
                if e2 is not e:
                    e.need((e2.sem, e2.n))
            for q in (self.SP, self.POOL, self.ACT):
                for i, s in enumerate(q.ring.sems):
                    e.need((s, q.ring.cnt[i] * 16))


class Rot:
    def __init__(self, K, n, shape, dt, psum=False):
        self.t = [(K.ps(shape, dt) if psum else K.sb(shape, dt), Res()) for _ in range(n)]
        self.i = 0

    def get(self):
        r = self.t[self.i]
        self.i = (self.i + 1) % len(self.t)
        return r


PV_CONVW, PV_CONVB, PV_BA, PV_BX, PV_LAM, PV_QN, PV_KN, PV_CQN, PV_CKN, PV_N = 0, 32, 40, 48, 56, 64, 65, 66, 67, 68
GV_MIX, GV_CX, GV_MEM, GV_FFN, GV_SUBLN, GV_LAMV, GV_BRG, GV_N = 0, 1024, 2048, 3072, 4096, 4224, 4480, 4516
C_IDF, C_ONES, C_BLK, C_ROT, C_TRI, C_INVF, C_EOFF, C_N = 0, 128, 256, 384, 512, 640, 641, 673
ROT_DIMS, HALF, THETA = 16, 8, 500000.0
TWO_PI = 2.0 * math.pi
CW1 = 6.28125
CW2 = float(np.float32(TWO_PI - CW1))
CW3 = float(TWO_PI - CW1 - CW2)
MAGIC = 12582912.0


def build(nseq, S, cap, upto=99):
    T = nseq * S
    NG = T // 512
    GS = S // 512
    NT = T // 128
    NSLOT = 32 * cap
    K = Kern()
    nc = K.nc
    PE, ACT, DVE, POOL, SP = K.PE, K.ACT, K.DVE, K.POOL, K.SP
    V, A, G, TE = nc.vector, nc.scalar, nc.gpsimd, nc.tensor

    def din(name, shape, dt=F32):
        return nc.dram_tensor(name, list(shape), dt, kind="ExternalInput").ap()

    x_d = din("x", [T, D])
    mem_d = din("mem", [nseq * 256, D])
    pos_d = din("pos", [nseq, S], I32)
    w_in_d = din("w_in", [D, 7168])
    pvec_d = din("pvec", [128, PV_N])
    gvec_d = din("gvec", [128, GV_N])
    cst_d = din("cst", [128, C_N])
    wa_d = din("lru_wa", [8, 128, 128])
    wx_d = din("lru_wx", [8, 128, 128])
    wlo_d = din("w_lru_o", [D, D])
    wao_d = din("w_attn_o", [D, D])
    wout_d = din("w_out", [D, D])
    wcq_d = din("w_cq", [D, 512])
    wckv_d = din("w_ckv", [D, D])
    wco_d = din("w_co", [512, D])
    wrg_d = din("w_rg", [D, 36])
    wgu_d = din("w_gate_up", [32, D, D])
    wd_d = din("w_down", [32, 512, D])
    out_d = nc.dram_tensor("out", [T, D], F32, kind="ExternalOutput").ap()
    cnt_d = nc.dram_tensor("cnt", [128, 32], F32, kind="ExternalOutput").ap()
    hT_d = nc.dram_tensor("hT_s", [8, 128, T], BF16).ap()
    q_d = nc.dram_tensor("q_s", [8, 128, T], BF16).ap()
    k_d = nc.dram_tensor("k_s", [8, 128, T], BF16).ap()
    v_d = nc.dram_tensor("v_s", [T, D], BF16).ap()
    xs_d = nc.dram_tensor("xs_s", [NSLOT, D], BF16).ap()
    ys_d = nc.dram_tensor("ys_s", [NSLOT, D], BF16).ap()

    def kc(ap):
        return ap.rearrange("(kc p) n -> p kc n", p=128)

    cst = K.sb([128, C_N], F32); CST = Res()
    pvec = K.sb([128, PV_N], F32); PVEC = Res()
    identb = K.sb([128, 128], BF16); IDB = Res()
    onesb = K.sb([128, 128], BF16)
    neglam = K.sb([128, 1], F32); NEGLAM = Res()
    cl = K.sb([128, 16], F32); CL = Res()
    wout = K.sb([128, 8, D], BF16); WOUT = Res()
    SP.dma(cst[:], cst_d, outs=[CST])
    SP.dma(pvec[:], pvec_d, outs=[PVEC])
    POOL.dma(wout[:], kc(wout_d), outs=[WOUT])
    DVE.op(lambda: V.tensor_copy(identb[:], cst[:, C_IDF:C_IDF + 128]), outs=[IDB], ins=[CST])
    DVE.op(lambda: V.tensor_copy(onesb[:], cst[:, C_ONES:C_ONES + 128]), outs=[IDB], ins=[CST])
    blkb = K.sb([128, 128], BF16)
    rotb = K.sb([128, 128], BF16)
    DVE.op(lambda: V.tensor_copy(blkb[:], cst[:, C_BLK:C_BLK + 128]), outs=[IDB], ins=[CST])
    DVE.op(lambda: V.tensor_copy(rotb[:], cst[:, C_ROT:C_ROT + 128]), outs=[IDB], ins=[CST])
    identf = cst[:, C_IDF:C_IDF + 128]
    onesf = cst[:, C_ONES:C_ONES + 128]
    blkf = cst[:, C_BLK:C_BLK + 128]
    rotf = cst[:, C_ROT:C_ROT + 128]

    def pcol(i):
        return pvec[:, i:i + 1]

    with ExitStack() as sc:
        K.scope = sc
        lamv = K.sb([128, 256], F32); LV = Res()
        tmp = K.sb([128, 128], F32); TMP = Res()
        s2 = K.sb([128, 4], F32); S2 = Res()
        SP.dma(lamv[:], gvec_d[:, GV_LAMV:GV_LAMV + 256], outs=[LV])
        DVE.op(lambda: V.tensor_tensor(tmp[:, 0:64], lamv[:, 0:64], lamv[:, 64:128], op=ALU.mult), outs=[TMP], ins=[LV])
        DVE.op(lambda: V.reduce_sum(s2[:, 0:1], tmp[:, 0:64], axis=AX.X), outs=[S2], ins=[TMP])
        DVE.op(lambda: V.tensor_tensor(tmp[:, 64:128], lamv[:, 128:192], lamv[:, 192:256], op=ALU.mult), outs=[TMP], ins=[LV])
        DVE.op(lambda: V.reduce_sum(s2[:, 1:2], tmp[:, 64:128], axis=AX.X), outs=[S2], ins=[TMP])
        ACT.op(lambda: A.activation(s2[:, 2:4], s2[:, 0:2], AF.Exp), outs=[S2], ins=[S2])
        DVE.op(lambda: V.tensor_tensor(s2[:, 0:1], s2[:, 3:4], s2[:, 2:3], op=ALU.subtract), outs=[S2], ins=[S2])
        DVE.op(lambda: V.tensor_scalar(neglam[:], s2[:, 0:1], -0.2, None, op0=ALU.add), outs=[NEGLAM], ins=[S2])
        ACT.op(lambda: A.activation(tmp[:, 0:8], pvec[:, PV_LAM:PV_LAM + 8], AF.Exp, scale=-1.0), outs=[TMP], ins=[PVEC])
        ACT.op(lambda: A.activation(tmp[:, 8:16], tmp[:, 0:8], AF.Ln, bias=1.0), outs=[TMP], ins=[TMP])
        DVE.op(lambda: V.tensor_scalar(cl[:, 0:8], tmp[:, 8:16], -8.0, None, op0=ALU.mult), outs=[CL], ins=[TMP])
        DVE.op(lambda: V.tensor_scalar(cl[:, 8:16], tmp[:, 8:16], -16.0, None, op0=ALU.mult), outs=[CL], ins=[TMP])
        K.barrier()
    K.scope = K.es

    def rmsnorm_tile(xt, XT, gb, GB, hb, HB, junk, JK, st, ST, out_f32=None):
        ACT.op(lambda: A.activation(junk[:], xt, AF.Square, accum_out=st[:, 0:1]), outs=[JK, ST], ins=[XT])
        ACT.op(lambda: A.activation(st[:, 1:2], st[:, 0:1], AF.Ln, scale=1.0 / D, bias=EPS), outs=[ST], ins=[ST])
        ACT.op(lambda: A.activation(st[:, 2:3], st[:, 1:2], AF.Exp, scale=-0.5), outs=[ST], ins=[ST])
        DVE.op(lambda: V.scalar_tensor_tensor(hb, xt, st[:, 2:3], gb, op0=ALU.mult, op1=ALU.mult), outs=[HB], ins=[XT, ST, GB])

    def transpose8(src, SRC, pt, PT, dst, DST, eng, ncols=8, ident=None):
        idn = identb[:] if ident is None else ident
        for c in range(ncols):
            PE.op(lambda c=c: TE.transpose(pt[:, c, :], src[:, c * 128:(c + 1) * 128], idn), outs=[PT], ins=[SRC, IDB], inc=(c == ncols - 1))
        if eng is ACT:
            ACT.op(lambda: A.copy(dst, pt[:, 0:ncols, :]), outs=[DST], ins=[PT])
        else:
            DVE.op(lambda: V.tensor_copy(dst, pt[:, 0:ncols, :]), outs=[DST], ins=[PT])

    def acc_mm(ps_ap, PSR, pairs, ins):
        n = len(pairs)
        for i, (l, r) in enumerate(pairs):
            PE.op(lambda l=l, r=r, i=i: TE.matmul(ps_ap, l, r, start=(i == 0), stop=(i == n - 1)), outs=[PSR], ins=ins, inc=(i == n - 1))

    def phase_norm_T(src_d, gcol, dst_d, ntiles_total):
        with ExitStack() as sc:
            K.scope = sc
            gb = K.sb([128, D], F32); GB = Res()
            SP.dma(gb[:], gvec_d[:, gcol:gcol + D], outs=[GB])
            xr = Rot(K, 3, [128, D], F32)
            hbr = Rot(K, 2, [128, D], BF16)
            junk = K.sb([128, D], F32); JK = Res()
            str_ = Rot(K, 4, [128, 4], F32)
            ptr = Rot(K, 2, [128, 8, 128], BF16, psum=True)
            hTr = Rot(K, 2, [128, 8, 512], BF16)
            for g in range(ntiles_total // 4):
                hT, HT = hTr.get()
                for j in range(4):
                    t = g * 4 + j
                    xt, XT = xr.get()
                    SP.dma(xt[:], src_d[t * 128:(t + 1) * 128, :], outs=[XT])
                    hb, HB = hbr.get()
                    st, ST = str_.get()
                    rmsnorm_tile(xt[:], XT, gb[:], GB, hb[:], HB, junk, JK, st, ST)
                    pt, PT = ptr.get()
                    transpose8(hb, HB, pt, PT, hT[:, :, j * 128:(j + 1) * 128], HT, ACT if j % 2 else DVE)
                POOL.dma(dst_d[:, :, g * 512:(g + 1) * 512].rearrange("kc p t -> p kc t"), hT[:], ins=[HT])
            K.barrier()
        K.scope = K.es

    phase_norm_T(x_d, GV_MIX, hT_d, NT)

    z_d = nc.dram_tensor("z_s", [8, 128, T], BF16).ap()

    def post_mix(g, hT, HT, ybuild, wg, WG, sgr, zbuf, ZB, psr, xr, resid_d, mode="both", zlr=None):
        for fo in range(8):
            py, PY = psr.get()
            ybuild(fo, py, PY)
            pg, PG = psr.get()
            acc_mm(pg[:], PG, [(wg[:, k, fo * 128:(fo + 1) * 128], hT[:, k, :]) for k in range(8)], [WG, HT])
            sg, SG = sgr.get()
            ACT.op(lambda: A.activation(sg[:], pg[:], AF.Sigmoid), outs=[SG], ins=[PG])
            if mode == "add":
                zl, ZL = zlr.get()
                SP.dma(zl[:], z_d[fo, :, g * 512:(g + 1) * 512], outs=[ZL])
            DVE.op(lambda: V.tensor_tensor(zbuf[:, fo, :], sg[:], py[:], op=ALU.mult), outs=[ZB], ins=[SG, PY])
            if mode == "add":
                DVE.op(lambda: V.tensor_tensor(zbuf[:, fo, :], zbuf[:, fo, :], zl[:], op=ALU.add), outs=[ZB], ins=[ZB, ZL])
        if mode == "store":
            POOL.dma(z_d[:, :, g * 512:(g + 1) * 512].rearrange("kc p t -> p kc t"), zbuf[:], ins=[ZB])
            return
        for tt in range(4):
            t = g * 4 + tt
            xt, XT = xr.get()
            SP.dma(xt[:], resid_d[t * 128:(t + 1) * 128, :], outs=[XT])
            for hf in range(2):
                po, PO = psr.get()
                acc_mm(po[:], PO, [(zbuf[:, k, tt * 128:(tt + 1) * 128], wout[:, k, hf * 512:(hf + 1) * 512]) for k in range(8)], [ZB, WOUT])
                DVE.op(lambda: V.tensor_tensor(xt[:, hf * 512:(hf + 1) * 512], xt[:, hf * 512:(hf + 1) * 512], po[:], op=ALU.add), outs=[XT], ins=[XT, PO])
            POOL.dma(out_d[t * 128:(t + 1) * 128, :], xt[:], ins=[XT])

    m_d = nc.dram_tensor("m_s", [8, 128, T], BF16).ap()
    if upto >= 1:
        with ExitStack() as sc:
            K.scope = sc
            wl = K.sb([128, 8, 2048], BF16); WL = Res()
            wa = K.sb([128, 8, 128], BF16); WA = Res()
            wx = K.sb([128, 8, 128], BF16)
            POOL.dma(wl[:], kc(w_in_d[:, 0:2048]), outs=[WL])
            POOL.dma(wa[:], wa_d.rearrange("n c d -> c n d"), outs=[WA])
            POOL.dma(wx[:], wx_d.rearrange("n c d -> c n d"), outs=[WA])
            hTr = Rot(K, 2, [128, 8, 512], BF16)
            xin = K.sb([128, 8, 515], F32); XIN = Res()
            xc = K.sb([128, 8, 512], F32); XC = [Res() for _ in range(8)]
            xcb = K.sb([128, 8, 512], BF16); XCB = [Res() for _ in range(8)]
            rr = K.sb([128, 8, 512], F32); RR = [Res() for _ in range(8)]
            ii = K.sb([128, 8, 512], F32); II = [Res() for _ in range(8)]
            aa = K.sb([128, 8, 512], F32); AA = [Res() for _ in range(8)]
            CURS = [Res() for _ in range(8)]
            hst = K.sb([128, 8], F32); HST = Res()
            mbr = Rot(K, 2, [128, 8, 512], BF16)
            glr = Rot(K, 3, [128, 512], F32)
            psr = Rot(K, 8, [128, 512], F32, psum=True)
            cur = xin
            for g in range(NG):
                b, gi = divmod(g, GS)
                mbuf, MB = mbr.get()
                if gi == 0:
                    DVE.op(lambda: V.memset(cur[:, :, 0:3], 0.0), outs=CURS)
                    DVE.op(lambda: V.memset(hst[:], 0.0), outs=[HST])
                hT, HT = hTr.get()
                SP.dma(hT[:], hT_d[:, :, g * 512:(g + 1) * 512].rearrange("kc p t -> p kc t"), outs=[HT])
                for c in range(8):
                    ps, PS = psr.get()
                    acc_mm(ps[:], PS, [(wl[:, k, c * 128:(c + 1) * 128], hT[:, k, :]) for k in range(8)], [WL, HT])
                    ACT.op(lambda: A.copy(cur[:, c, 3:515], ps[:]), outs=[CURS[c]], ins=[PS])
                    cw = lambda j: pcol(PV_CONVW + c * 4 + j)
                    ACT.op(lambda: A.activation(xc[:, c, :], ps[:], AF.Identity, scale=cw(3), bias=pcol(PV_CONVB + c)), outs=[XC[c]], ins=[PS, PVEC])
                    for j in range(3):
                        DVE.op(lambda j=j: V.scalar_tensor_tensor(xc[:, c, :], cur[:, c, j:j + 512], cw(j), xc[:, c, :], op0=ALU.mult, op1=ALU.add), outs=[XC[c]], ins=[CURS[c], PVEC, XC[c]])
                    POOL.op(lambda: G.tensor_copy(xcb[:, c, :], xc[:, c, :]), outs=[XCB[c]], ins=[XC[c]])
                    POOL.op(lambda: G.tensor_copy(cur[:, c, 0:3], cur[:, c, 512:515]), outs=[CURS[c]], ins=[CURS[c]])
                for c in range(8):
                    ps, PS = psr.get()
                    PE.op(lambda: TE.matmul(ps[:], wa[:, c, :], xcb[:, c, :], start=True, stop=True), outs=[PS], ins=[WA, XCB[c]])
                    ACT.op(lambda: A.activation(rr[:, c, :], ps[:], AF.Sigmoid, bias=pcol(PV_BA + c)), outs=[RR[c]], ins=[PS, PVEC])
                    ps2, PS2 = psr.get()
                    PE.op(lambda: TE.matmul(ps2[:], wx[:, c, :], xcb[:, c, :], start=True, stop=True), outs=[PS2], ins=[WA, XCB[c]])
                    ACT.op(lambda: A.activation(ii[:, c, :], ps2[:], AF.Sigmoid, bias=pcol(PV_BX + c)), outs=[II[c]], ins=[PS2, PVEC])
                for c in range(8):
                    ACT.op(lambda: A.activation(aa[:, c, :], rr[:, c, :], AF.Exp, scale=cl[:, c:c + 1]), outs=[AA[c]], ins=[RR[c], CL])
                    ACT.op(lambda: A.activation(rr[:, c, :], rr[:, c, :], AF.Exp, scale=cl[:, 8 + c:9 + c]), outs=[RR[c]], ins=[RR[c], CL])
                for c in range(8):
                    ACT.op(lambda: A.activation(rr[:, c, :], rr[:, c, :], AF.Sqrt, scale=-1.0, bias=1.0), outs=[RR[c]], ins=[RR[c]])
                    POOL.op(lambda: G.tensor_tensor(ii[:, c, :], ii[:, c, :], xc[:, c, :], op=ALU.mult), outs=[II[c]], ins=[II[c], XC[c]])
                    DVE.op(lambda: V.tensor_tensor(ii[:, c, :], ii[:, c, :], rr[:, c, :], op=ALU.mult), outs=[II[c]], ins=[II[c], RR[c]])
                    DVE.op(lambda: V.tensor_tensor_scan(xc[:, c, :], aa[:, c, :], ii[:, c, :], hst[:, c:c + 1], op0=ALU.mult, op1=ALU.add), outs=[XC[c]], ins=[AA[c], II[c], HST])
                    DVE.op(lambda: V.tensor_copy(hst[:, c:c + 1], xc[:, c, 511:512]), outs=[HST], ins=[XC[c]])
                for c in range(8):
                    ps, PS = psr.get()
                    acc_mm(ps[:], PS, [(wl[:, k, 1024 + c * 128:1024 + (c + 1) * 128], hT[:, k, :]) for k in range(8)], [WL, HT])
                    gl, GL = glr.get()
                    ACT.op(lambda: A.activation(gl[:], ps[:], AF.Gelu), outs=[GL], ins=[PS])
                    POOL.op(lambda: G.tensor_tensor(mbuf[:, c, :], gl[:], xc[:, c, :], op=ALU.mult), outs=[MB], ins=[GL, XC[c]])
                POOL.dma(m_d[:, :, g * 512:(g + 1) * 512].rearrange("kc p t -> p kc t"), mbuf[:], ins=[MB])
            K.barrier()
        K.scope = K.es
        with ExitStack() as sc:
            K.scope = sc
            wg = K.sb([128, 8, D], BF16); WG = Res()
            wlo = K.sb([128, 8, D], BF16); WLO = Res()
            POOL.dma(wg[:], kc(w_in_d[:, 5120:6144]), outs=[WG])
            POOL.dma(wlo[:], kc(wlo_d), outs=[WLO])
            hTr = Rot(K, 3, [128, 8, 512], BF16)
            mr = Rot(K, 3, [128, 8, 512], BF16)
            zbr = Rot(K, 2, [128, 8, 512], BF16)
            sgr = Rot(K, 3, [128, 512], F32)
            xr = Rot(K, 4, [128, D], F32)
            psr = Rot(K, 8, [128, 512], F32, psum=True)
            for g in range(NG):
                hT, HT = hTr.get()
                SP.dma(hT[:], hT_d[:, :, g * 512:(g + 1) * 512].rearrange("kc p t -> p kc t"), outs=[HT])
                mb_, MB_ = mr.get()
                SP.dma(mb_[:], m_d[:, :, g * 512:(g + 1) * 512].rearrange("kc p t -> p kc t"), outs=[MB_])
                zbuf, ZB = zbr.get()

                def ybuild(fo, py, PY):
                    acc_mm(py[:], PY, [(wlo[:, k, fo * 128:(fo + 1) * 128], mb_[:, k, :]) for k in range(8)], [WLO, MB_])
                post_mix(g, hT, HT, ybuild, wg, WG, sgr, zbuf, ZB, psr, xr, x_d, mode=("store" if upto >= 3 else "both"))
            K.barrier()
        K.scope = K.es

    if upto >= 2:
        with ExitStack() as sc:
            K.scope = sc
            wqk = K.sb([128, 8, 2048], BF16); WQK = Res()
            wv = K.sb([128, 8, D], BF16); WV = Res()
            POOL.dma(wqk[:], kc(w_in_d[:, 2048:4096]), outs=[WQK])
            POOL.dma(wv[:], kc(w_in_d[:, 4096:5120]), outs=[WV])
            TC = min(S, 1024)
            posi = K.sb([128, TC], I32); POSI = Res()
            ang = K.sb([128, TC], F32); ANG = Res()
            nn = K.sb([128, TC], F32); NN = Res()
            cosT = K.sb([128, S], F32); COS = Res()
            sinT = K.sb([128, S], F32); SIN = Res()
            hTr = Rot(K, 2, [128, 8, 512], BF16)
            psr = Rot(K, 8, [128, 512], F32, psum=True)
            sqr = Rot(K, 4, [128, 512], BF16)
            rsr = Rot(K, 4, [128, 512], F32)
            qnr = Rot(K, 4, [128, 512], BF16)
            t1r = Rot(K, 4, [128, 512], F32)
            t2r = Rot(K, 4, [128, 512], F32)
            qor = Rot(K, 4, [128, 512], BF16)
            vtr = Rot(K, 2, [128, D], BF16)
            gq = K.sb([128, 2], F32); GQ = Res()
            DVE.op(lambda: V.tensor_scalar(gq[:, 0:1], pcol(PV_QN), 0.125, None, op0=ALU.mult), outs=[GQ], ins=[PVEC])
            DVE.op(lambda: V.tensor_copy(gq[:, 1:2], pcol(PV_KN)), outs=[GQ], ins=[PVEC])
            for b in range(nseq):
                for t0 in range(0, S, TC):
                    csl = slice(t0, t0 + TC)
                    SP.dma(posi[:], pos_d[b:b + 1, csl].partition_broadcast(128), outs=[POSI])
                    DVE.op(lambda: V.tensor_copy(ang[:], posi[:]), outs=[ANG], ins=[POSI])
                    DVE.op(lambda: V.tensor_scalar(ang[:], ang[:], cst[:, C_INVF:C_INVF + 1], None, op0=ALU.mult), outs=[ANG], ins=[ANG, CST])
                    DVE.op(lambda: V.tensor_scalar(nn[:], ang[:], 1.0 / TWO_PI, MAGIC, op0=ALU.mult, op1=ALU.add), outs=[NN], ins=[ANG])
                    DVE.op(lambda: V.tensor_scalar(nn[:], nn[:], -MAGIC, None, op0=ALU.add), outs=[NN], ins=[NN])
                    for cw in (CW1, CW2, CW3):
                        DVE.op(lambda cw=cw: V.scalar_tensor_tensor(ang[:], nn[:], -cw, ang[:], op0=ALU.mult, op1=ALU.add), outs=[ANG], ins=[NN, ANG])
                    DVE.op(lambda: V.tensor_scalar(ang[:], ang[:], math.pi, -math.pi, op0=ALU.min, op1=ALU.max), outs=[ANG], ins=[ANG])
                    ACT.op(lambda: A.activation(sinT[:, csl], ang[:], AF.Sin), outs=[SIN], ins=[ANG])
                    DVE.op(lambda: V.tensor_scalar(ang[:], ang[:], math.pi / 2, None, op0=ALU.add), outs=[ANG], ins=[ANG])
                    DVE.op(lambda: V.tensor_scalar(nn[:], ang[:], math.pi, -TWO_PI, op0=ALU.is_gt, op1=ALU.mult), outs=[NN], ins=[ANG])
                    DVE.op(lambda: V.tensor_tensor(ang[:], ang[:], nn[:], op=ALU.add), outs=[ANG], ins=[ANG, NN])
                    DVE.op(lambda: V.tensor_scalar(ang[:], ang[:], math.pi, -math.pi, op0=ALU.min, op1=ALU.max), outs=[ANG], ins=[ANG])
                    ACT.op(lambda: A.activation(cosT[:, csl], ang[:], AF.Sin), outs=[COS], ins=[ANG])
                for gi in range(GS):
                    g = b * GS + gi
                    tsl = slice(gi * 512, (gi + 1) * 512)
                    hT, HT = hTr.get()
                    SP.dma(hT[:], hT_d[:, :, g * 512:(g + 1) * 512].rearrange("kc p t -> p kc t"), outs=[HT])
                    units = [(qk, h) for qk in range(2) for h in range(8)]
                    for b0 in range(0, 16, 4):
                        bu = units[b0:b0 + 4]
                        st = []
                        for (qk, h) in bu:
                            c0 = qk * 1024 + h * 128
                            ps, PS = psr.get()
                            acc_mm(ps[:], PS, [(wqk[:, k, c0:c0 + 128], hT[:, k, :]) for k in range(8)], [WQK, HT])
                            st.append(dict(qk=qk, h=h, ps=ps, PS=PS))
                        for u in st:
                            u["sq"], u["SQ"] = sqr.get()
                            ACT.op(lambda: A.activation(u["sq"][:], u["ps"][:], AF.Square), outs=[u["SQ"]], ins=[u["PS"]])
                        for u in st:
                            u["pss"], u["PSS"] = psr.get()
                            PE.op(lambda: TE.matmul(u["pss"][:], blkb[:], u["sq"][:], start=True, stop=True), outs=[u["PSS"]], ins=[IDB, u["SQ"]])
                        for u in st:
                            u["rs"], u["RS"] = rsr.get()
                            ACT.op(lambda: A.activation(u["rs"][:], u["pss"][:], AF.Ln, scale=1.0 / 64, bias=EPS), outs=[u["RS"]], ins=[u["PSS"]])
                        for u in st:
                            ACT.op(lambda: A.activation(u["rs"][:], u["rs"][:], AF.Exp, scale=-0.5), outs=[u["RS"]], ins=[u["RS"]])
                        for u in st:
                            u["qn"], u["QN"] = qnr.get()
                            DVE.op(lambda: V.scalar_tensor_tensor(u["qn"][:], u["ps"][:], gq[:, u["qk"]:u["qk"] + 1], u["rs"][:], op0=ALU.mult, op1=ALU.mult), outs=[u["QN"]], ins=[u["PS"], GQ, u["RS"]])
                        for u in st:
                            u["pr"], u["PR"] = psr.get()
                            PE.op(lambda: TE.matmul(u["pr"][:], rotb[:], u["qn"][:], start=True, stop=True), outs=[u["PR"]], ins=[IDB, u["QN"]])
                        for u in st:
                            u["t1"], u["T1"] = t1r.get()
                            POOL.op(lambda: G.tensor_tensor(u["t1"][:], u["qn"][:], cosT[:, tsl], op=ALU.mult), outs=[u["T1"]], ins=[u["QN"], COS])
                        for u in st:
                            u["t2"], u["T2"] = t2r.get()
                            DVE.op(lambda: V.tensor_tensor(u["t2"][:], u["pr"][:], sinT[:, tsl], op=ALU.mult), outs=[u["T2"]], ins=[u["PR"], SIN])
                        for u in st:
                            qo, QO = qor.get()
                            POOL.op(lambda: G.tensor_tensor(qo[:], u["t1"][:], u["t2"][:], op=ALU.add), outs=[QO], ins=[u["T1"], u["T2"]])
                            dst = q_d if u["qk"] == 0 else k_d
                            POOL.dma(dst[u["h"], :, g * 512:(g + 1) * 512], qo[:], ins=[QO])
                    for tt in range(4):
                        t = g * 4 + tt
                        vt, VT = vtr.get()
                        for hf in range(2):
                            ps, PS = psr.get()
                            acc_mm(ps[:], PS, [(hT[:, k, tt * 128:(tt + 1) * 128], wv[:, k, hf * 512:(hf + 1) * 512]) for k in range(8)], [HT, WV])
                            ACT.op(lambda: A.copy(vt[:, hf * 512:(hf + 1) * 512], ps[:]), outs=[VT], ins=[PS])
                        POOL.dma(v_d[t * 128:(t + 1) * 128, :], vt[:], ins=[VT])
            K.barrier()
        K.scope = K.es

    if upto >= 3:
        with ExitStack() as sc:
            K.scope = sc
            NQT = S // 128
            wao = K.sb([128, 8, D], BF16); WAO = Res()
            wg = K.sb([128, 8, D], BF16); WG = Res()
            POOL.dma(wao[:], kc(wao_d), outs=[WAO])
            POOL.dma(wg[:], kc(w_in_d[:, 6144:7168]), outs=[WG])
            sub_b = K.sb([128, 128], F32); SUBB = Res()
            SP.dma(sub_b[:], gvec_d[:, GV_SUBLN:GV_SUBLN + 128], outs=[SUBB])
            DVE.op(lambda: V.tensor_scalar(sub_b[:], sub_b[:], 0.8, None, op0=ALU.mult), outs=[SUBB], ins=[SUBB])
            oT = K.sb([128, 8, S], BF16); OT = Res()
            qTr = Rot(K, 2, [128, S], BF16)
            kTr = Rot(K, 2, [128, S], BF16)
            var = Rot(K, 2, [128, NQT, 132], BF16)
            for va, VA in var.t:
                POOL.op(lambda va=va: G.memset(va[:, :, 128:129], 1.0), outs=[VA])
            pscr = [(K.ps([128, 2, 512], F32), [Res(), Res()]) for _ in range(2)]
            pscr_i = [0]

            def get_sc():
                r = pscr[pscr_i[0]]
                pscr_i[0] ^= 1
                return r
            paccb = [(K.ps([128, 512], F32), Res()) for _ in range(3)]
            ptp = (K.ps([128, 512], F32), Res())

            def acs(m, j):
                idx = m * 4 + j
                t_, R_ = paccb[idx // 3]
                return t_, R_, (idx % 3) * 132, (idx % 3 == 0)
            bankviews = _Views([(t[:, m, :], RS[m]) for (t, RS) in pscr for m in range(2)])
            Pr = Rot(K, 3, [128, 2, 512], BF16)
            smr = Rot(K, 2, [128, 8, 4], F32)
            onr = Rot(K, 1, [128, 8, 128], F32)
            o1r = Rot(K, 2, [128, 4, 128], F32)
            jkr = Rot(K, 2, [128, 4, 128], F32)
            hTr = Rot(K, 1, [128, 8, 512], BF16)
            zbuf = K.sb([128, 8, 512], BF16); ZB = Res()
            sgr = Rot(K, 1, [128, 512], F32)
            xr = Rot(K, 1, [128, D], F32)
            zlr = Rot(K, 1, [128, 512], BF16)
            deferred = [None]
            heads = {}

            def get_head(b, h):
                if (b, h) not in heads:
                    qT, QT = qTr.get()
                    kT, KT = kTr.get()
                    va, VA = var.get()
                    SP.dma(qT[:], q_d[h, :, b * S:(b + 1) * S], outs=[QT])
                    SP.dma(kT[:], k_d[h, :, b * S:(b + 1) * S], outs=[KT])
                    SP.dma(va[:, :, 0:128], v_d[b * S:(b + 1) * S, h * 128:(h + 1) * 128].rearrange("(t p) d -> p t d", p=128), outs=[VA])
                    heads[(b, h)] = (qT, QT, kT, KT, va, VA)
                return heads[(b, h)]

            def issue_scores(step):
                b, h, qg, kt = step
                qT, QT, kT, KT, va, VA = get_head(b, h)
                jmin = max(0, kt - 4 * qg)
                ncol = (4 - jmin) * 128
                q0 = qg * 512 + jmin * 128
                sp_, RS = get_sc()
                for m in range(2):
                    msl = slice(m * 64, (m + 1) * 64)
                    PE.op(lambda: TE.matmul(sp_[:, m, 0:ncol], kT[msl, kt * 128:(kt + 1) * 128], qT[msl, q0:q0 + ncol], start=True, stop=True),
                          outs=[RS[m]], ins=[KT, QT], inc=(m == 1))
                return sp_, RS, jmin, ncol

            for b in range(nseq):
                steps = [(b, h, qg, kt) for h in range(8) for qg in range(GS) for kt in range(4 * qg + 4)]
                pend = [issue_scores(steps[0]), issue_scores(steps[1])]
                for i, (b_, h, qg, kt) in enumerate(steps):
                    if qg == 0 and kt == 0 and h + 1 < 8:
                        get_head(b, h + 1)
                    qT, QT, kT, KT, va, VA = get_head(b, h)
                    nkt = 4 * qg + 4
                    sp_, RS, jmin, ncol = pend.pop(0)
                    P, PR = Pr.get()
                    ACT.op(lambda: A.activation(P[:, :, 0:ncol], sp_[:, :, 0:ncol], AF.Exp), outs=[PR], ins=RS)
                    if kt >= 4 * qg:
                        POOL.op(lambda: G.memset(P[64:128, :, 0:64], 0.0), outs=[PR], ins=[PR])
                    if i + 2 < len(steps):
                        pend.append(issue_scores(steps[i + 2]))
                    for m in range(2):
                        for j in range(jmin, 4):
                            ac, AC, col, bank_first = acs(m, j)
                            first = (kt == 0 and bank_first)
                            last = (kt == 4 * qg + j)
                            PE.op(lambda: TE.matmul(ac[:, col:col + 129], P[:, m, (j - jmin) * 128:(j - jmin + 1) * 128], va[:, kt, 0:129], start=first, stop=last),
                                  outs=[AC], ins=[PR, VA], inc=(m == 1 and j == 3))
                    if kt == 3 and deferred[0] is not None:
                        deferred[0]()
                        deferred[0] = None
                    if kt != nkt - 1:
                        continue
                    sm, SM = smr.get()
                    o1, O1 = o1r.get()
                    on, ON = onr.get()
                    jk, JK = jkr.get()
                    for bk in range(3):
                        na = 3 if bk < 2 else 2
                        t_, R_ = paccb[bk]
                        v3 = t_[:, 0:na * 132].rearrange("p (a c) -> p a c", c=132)
                        rc = sm[:, 0:2, :].rearrange("p a b -> p (a b)")[:, bk * 3:bk * 3 + na]
                        DVE.op(lambda: V.reciprocal(rc, v3[:, :, 128]), outs=[SM], ins=[R_])
                        DVE.op(lambda: V.tensor_tensor(on[:, bk * 3:bk * 3 + na, :], v3[:, :, 0:128], rc.unsqueeze(2).to_broadcast([128, na, 128]), op=ALU.mult),
                               outs=[ON], ins=[R_, SM])
                    DVE.op(lambda: V.scalar_tensor_tensor(o1[:], on[:, 4:8, :], neglam[:, 0:1], on[:, 0:4, :], op0=ALU.mult, op1=ALU.add), outs=[O1], ins=[ON, NEGLAM])
                    DVE.op(lambda: V.tensor_tensor(jk[:], o1[:], o1[:], op=ALU.mult), outs=[JK], ins=[O1])
                    DVE.op(lambda: V.reduce_sum(sm[:, 3, :], jk[:], axis=AX.X), outs=[SM], ins=[JK])

                    def part_b(sm=sm, SM=SM, o1=o1, O1=O1, jk=jk, JK=JK, h=h, qg=qg):
                        ACT.op(lambda: A.activation(sm[:, 4, :], sm[:, 3, :], AF.Ln, scale=1.0 / 128, bias=EPS), outs=[SM], ins=[SM])
                        ACT.op(lambda: A.activation(sm[:, 5, :], sm[:, 4, :], AF.Exp, scale=-0.5), outs=[SM], ins=[SM])
                        for j in range(4):
                            DVE.op(lambda: V.scalar_tensor_tensor(jk[:, j, :], o1[:, j, :], sm[:, 5, j:j + 1], sub_b[:], op0=ALU.mult, op1=ALU.mult), outs=[JK], ins=[O1, SM, SUBB])
                        tp_, TP = ptp
                        for j in range(4):
                            PE.op(lambda: TE.transpose(tp_[:, j * 128:(j + 1) * 128], jk[:, j, :], identf), outs=[TP], ins=[JK, CST], inc=(j == 3))
                        DVE.op(lambda: V.tensor_copy(oT[:, h, qg * 512:(qg + 1) * 512], tp_[:, :]), outs=[OT], ins=[TP])
                    deferred[0] = part_b
                if deferred[0] is not None:
                    deferred[0]()
                    deferred[0] = None
                for gi in range(GS):
                    g = b * GS + gi
                    hT, HT = hTr.get()
                    SP.dma(hT[:], hT_d[:, :, g * 512:(g + 1) * 512].rearrange("kc p t -> p kc t"), outs=[HT])
                    def ybuild(fo, py, PY):
                        acc_mm(py[:], PY, [(wao[:, hh_, fo * 128:(fo + 1) * 128], oT[:, hh_, gi * 512:(gi + 1) * 512]) for hh_ in range(8)], [WAO, OT])
                    post_mix(g, hT, HT, ybuild, wg, WG, sgr, zbuf, ZB, bankviews, xr, x_d, mode="add", zlr=zlr)
            K.barrier()
        K.scope = K.es

    if upto >= 4:
        with ExitStack() as sc:
            K.scope = sc
            wcq = K.sb([128, 8, 512], BF16); WCQ = Res()
            wckv = K.sb([128, 8, D], BF16); WCKV = Res()
            wco = K.sb([128, 4, D], BF16); WCO = Res()
            POOL.dma(wcq[:], kc(wcq_d), outs=[WCQ])
            POOL.dma(wckv[:], kc(wckv_d), outs=[WCKV])
            POOL.dma(wco[:], kc(wco_d), outs=[WCO])
            gcx = K.sb([128, D], F32); GCX = Res()
            gmem = K.sb([128, D], F32); GMEM = Res()
            SP.dma(gcx[:], gvec_d[:, GV_CX:GV_CX + D], outs=[GCX])
            SP.dma(gmem[:], gvec_d[:, GV_MEM:GV_MEM + D], outs=[GMEM])
            gq = K.sb([128, 1], F32); GQ = Res()
            DVE.op(lambda: V.tensor_scalar(gq[:], pcol(PV_CQN), 128.0 ** -0.5, None, op0=ALU.mult), outs=[GQ], ins=[PVEC])
            xr = Rot(K, 5, [128, D], F32)
            hbr = Rot(K, 2, [128, D], BF16)
            junk = K.sb([128, D], F32); JK = Res()
            str_ = Rot(K, 4, [128, 4], F32)
            ptr = Rot(K, 2, [128, 8, 128], BF16, psum=True)
            psr = Rot(K, 6, [128, 512], F32, psum=True)
            memT = K.sb([128, 8, 256], BF16); MEMT = Res()
            kcT = K.sb([128, 4, 256], BF16); KCT = Res()
            vc = K.sb([128, 2, 512], BF16); VC = Res()
            hcT = K.sb([128, 8, 512], BF16); HCT = Res()
            sqr = Rot(K, 4, [128, 512], BF16)
            rsr = Rot(K, 8, [128, 512], F32)
            qnr = Rot(K, 4, [128, 512], BF16)
            qfr = Rot(K, 4, [128, 512], F32)
            Pr = Rot(K, 4, [128, 2, 512], BF16)
            ocT = K.sb([128, 4, 512], BF16); OCT = Res()
            for b in range(nseq):
                for mt in range(2):
                    xt, XT = xr.get()
                    SP.dma(xt[:], mem_d[b * 256 + mt * 128:b * 256 + (mt + 1) * 128, :], outs=[XT])
                    hb, HB = hbr.get()
                    st, ST = str_.get()
                    rmsnorm_tile(xt[:], XT, gmem[:], GMEM, hb[:], HB, junk, JK, st, ST)
                    pt, PT = ptr.get()
                    transpose8(hb, HB, pt, PT, memT[:, :, mt * 128:(mt + 1) * 128], MEMT, DVE)
                for h in range(4):
                    ps, PS = psr.get()
                    acc_mm(ps[:, 0:256], PS, [(wckv[:, k, h * 128:(h + 1) * 128], memT[:, k, :]) for k in range(8)], [WCKV, MEMT])
                    sq, SQ = sqr.get()
                    ACT.op(lambda: A.activation(sq[:, 0:256], ps[:, 0:256], AF.Square), outs=[SQ], ins=[PS])
                    pss, PSS = psr.get()
                    PE.op(lambda: TE.matmul(pss[:, 0:256], onesb[:], sq[:, 0:256], start=True, stop=True), outs=[PSS], ins=[IDB, SQ])
                    rs, RS = rsr.get()
                    ACT.op(lambda: A.activation(rs[:, 0:256], pss[:, 0:256], AF.Ln, scale=1.0 / 128, bias=EPS), outs=[RS], ins=[PSS])
                    ACT.op(lambda: A.activation(rs[:, 0:256], rs[:, 0:256], AF.Exp, scale=-0.5), outs=[RS], ins=[RS])
                    DVE.op(lambda: V.scalar_tensor_tensor(kcT[:, h, :], ps[:, 0:256], pcol(PV_CKN), rs[:, 0:256], op0=ALU.mult, op1=ALU.mult), outs=[KCT], ins=[PS, PVEC, RS])
                for mt in range(2):
                    ps, PS = psr.get()
                    acc_mm(ps[:], PS, [(memT[:, k, mt * 128:(mt + 1) * 128], wckv[:, k, 512:1024]) for k in range(8)], [MEMT, WCKV])
                    ACT.op(lambda: A.copy(vc[:, mt, :], ps[:]), outs=[VC], ins=[PS])
                for gi in range(GS):
                    g = b * GS + gi
                    xts = []
                    for tt in range(4):
                        t = g * 4 + tt
                        xt, XT = xr.get()
                        xts.append((xt, XT))
                        SP.dma(xt[:], out_d[t * 128:(t + 1) * 128, :], outs=[XT])
                        hb, HB = hbr.get()
                        st, ST = str_.get()
                        rmsnorm_tile(xt[:], XT, gcx[:], GCX, hb[:], HB, junk, JK, st, ST)
                        pt, PT = ptr.get()
                        transpose8(hb, HB, pt, PT, hcT[:, :, tt * 128:(tt + 1) * 128], HCT, ACT if tt % 2 else DVE)
                    hs = [dict(h=h) for h in range(4)]
                    for u in hs:
                        h = u["h"]
                        ps, PS = psr.get()
                        acc_mm(ps[:], PS, [(wcq[:, k, h * 128:(h + 1) * 128], hcT[:, k, :]) for k in range(8)], [WCQ, HCT])
                        u["qf"], u["QF"] = qfr.get()
                        ACT.op(lambda: A.copy(u["qf"][:], ps[:]), outs=[u["QF"]], ins=[PS])
                    for u in hs:
                        u["sq"], u["SQ"] = sqr.get()
                        ACT.op(lambda: A.activation(u["sq"][:], u["qf"][:], AF.Square), outs=[u["SQ"]], ins=[u["QF"]])
                    for u in hs:
                        u["pss"], u["PSS"] = psr.get()
                        PE.op(lambda: TE.matmul(u["pss"][:], onesb[:], u["sq"][:], start=True, stop=True), outs=[u["PSS"]], ins=[IDB, u["SQ"]])
                    for u in hs:
                        u["rs"], u["RS"] = rsr.get()
                        ACT.op(lambda: A.activation(u["rs"][:], u["pss"][:], AF.Ln, scale=1.0 / 128, bias=EPS), outs=[u["RS"]], ins=[u["PSS"]])
                    for u in hs:
                        ACT.op(lambda: A.activation(u["rs"][:], u["rs"][:], AF.Exp, scale=-0.5), outs=[u["RS"]], ins=[u["RS"]])
                    for u in hs:
                        u["qn"], u["QN"] = qnr.get()
                        DVE.op(lambda: V.scalar_tensor_tensor(u["qn"][:], u["qf"][:], gq[:, 0:1], u["rs"][:], op0=ALU.mult, op1=ALU.mult), outs=[u["QN"]], ins=[u["QF"], GQ, u["RS"]])
                    for u in hs:
                        h = u["h"]
                        u["P"], u["PRr"] = Pr.get()
                        for mt in range(2):
                            psc, PSC = psr.get()
                            PE.op(lambda: TE.matmul(psc[:], kcT[:, h, mt * 128:(mt + 1) * 128], u["qn"][:], start=True, stop=True), outs=[PSC], ins=[KCT, u["QN"]])
                            ACT.op(lambda: A.activation(u["P"][:, mt, :], psc[:], AF.Exp), outs=[u["PRr"]], ins=[PSC])
                    for u in hs:
                        h = u["h"]
                        po, PO = psr.get()
                        acc_mm(po[:], PO, [(vc[:, mt, h * 128:(h + 1) * 128], u["P"][:, mt, :]) for mt in range(2)], [VC, u["PRr"]])
                        psm, PSM = psr.get()
                        acc_mm(psm[:], PSM, [(onesb[:], u["P"][:, mt, :]) for mt in range(2)], [IDB, u["PRr"]])
                        rs2, RS2 = rsr.get()
                        DVE.op(lambda: V.reciprocal(rs2[:], psm[:]), outs=[RS2], ins=[PSM])
                        DVE.op(lambda: V.tensor_tensor(ocT[:, h, :], po[:], rs2[:], op=ALU.mult), outs=[OCT], ins=[PO, RS2])
                    for tt in range(4):
                        t = g * 4 + tt
                        xt, XT = xts[tt]
                        for hf in range(2):
                            po, PO = psr.get()
                            acc_mm(po[:], PO, [(ocT[:, hh_, tt * 128:(tt + 1) * 128], wco[:, hh_, hf * 512:(hf + 1) * 512]) for hh_ in range(4)], [OCT, WCO])
                            DVE.op(lambda: V.tensor_tensor(xt[:, hf * 512:(hf + 1) * 512], xt[:, hf * 512:(hf + 1) * 512], po[:], op=ALU.add), outs=[XT], ins=[XT, PO])
                        POOL.dma(out_d[t * 128:(t + 1) * 128, :], xt[:], ins=[XT])
            K.barrier()
        K.scope = K.es

    if upto >= 5:
        with ExitStack() as sc:
            K.scope = sc
            slots = K.sb([128, NT, 2], I32); SL = Res()
            wts = K.sb([128, NT, 2], F32); WT = Res()
            with ExitStack() as sc1:
                K.scope = sc1
                gff = K.sb([128, D], F32); GFF = Res()
                brg = K.sb([128, 36], F32); BRG = Res()
                wrg = K.sb([128, 8, 36], F32); WRG = Res()
                SP.dma(gff[:], gvec_d[:, GV_FFN:GV_FFN + D], outs=[GFF])
                SP.dma(brg[:], gvec_d[:, GV_BRG:GV_BRG + 36], outs=[BRG])
                SP.dma(wrg[:], kc(wrg_d), outs=[WRG])
                base = K.sb([128, 32], F32); BASE = Res()
                DVE.op(lambda: V.tensor_copy(base[:], cst[:, C_EOFF:C_EOFF + 32]), outs=[BASE], ins=[CST])
                xr = Rot(K, 3, [128, D], F32)
                hfr = Rot(K, 2, [128, D], F32)
                hbr = Rot(K, 10, [128, D], BF16)
                junk = K.sb([128, D], F32); JK = Res()
                str_ = Rot(K, 4, [128, 4], F32)
                ptf = Rot(K, 2, [128, 4, 128], F32, psum=True)
                hTf = Rot(K, 2, [128, 8, 128], F32)
                pl = Rot(K, 3, [128, 64], F32, psum=True)
                prk = Rot(K, 2, [128, 5, 32], F32, psum=True)
                lgr = Rot(K, 2, [128, 4, 36], F32)
                w1r = Rot(K, 2, [128, 16, 4], F32)
                ohgr = Rot(K, 2, [128, 4, 4], F32)
                d4r = Rot(K, 2, [128, 4, 4], F32)
                elr = Rot(K, 2, [128, 3, 4, 8], F32)
                mkr = Rot(K, 2, [128, 2, 4, 8], F32)
                prr = Rot(K, 2, [128, 4, 32], F32)
                ohr = Rot(K, 2, [128, 3, 4, 32], F32)
                ohb = Rot(K, 2, [128, 4, 32], BF16)
                rkr = Rot(K, 2, [128, 4, 32], F32)
                trib = K.sb([128, 128], BF16); TRIB = Res()
                DVE.op(lambda: V.tensor_copy(trib[:], cst[:, C_TRI:C_TRI + 128]), outs=[TRIB], ins=[CST])
                bc = lambda ap, shape, ax: ap.unsqueeze(ax).to_broadcast(shape)
                for tb in range(NT // 4):
                    lg, LG = lgr.get()
                    hbs = []
                    for ti in range(4):
                        t = tb * 4 + ti
                        xt, XT = xr.get()
                        SP.dma(xt[:], out_d[t * 128:(t + 1) * 128, :], outs=[XT])
                        hf, HF = hfr.get()
                        st, ST = str_.get()
                        rmsnorm_tile(xt[:], XT, gff[:], GFF, hf[:], HF, junk, JK, st, ST)
                        hb, HB = hbr.get()
                        hbs.append((hb, HB))
                        ACT.op(lambda: A.copy(hb[:], hf[:]), outs=[HB], ins=[HF])
                        hT, HT = hTf.get()
                        for half in range(2):
                            pt, PT = ptf.get()
                            for c in range(4):
                                cc = half * 4 + c
                                PE.op(lambda: TE.transpose(pt[:, c, :], hf[:, cc * 128:(cc + 1) * 128], identf), outs=[PT], ins=[HF, CST], inc=(c == 3))
                            if half:
                                ACT.op(lambda: A.copy(hT[:, 4:8, :], pt[:]), outs=[HT], ins=[PT])
                            else:
                                DVE.op(lambda: V.tensor_copy(hT[:, 0:4, :], pt[:]), outs=[HT], ins=[PT])
                        lp, LP = pl.get()
                        acc_mm(lp[:, 0:36], LP, [(hT[:, k, :], wrg[:, k, :]) for k in range(8)], [HT, WRG])
                        DVE.op(lambda: V.tensor_tensor(lg[:, ti, :], lp[:, 0:36], brg[:], op=ALU.add), outs=[LG], ins=[LP, BRG])
                    w1, W1 = w1r.get()
                    ohg, OHG = ohgr.get()
                    d4, D4 = d4r.get()
                    el, EL = elr.get()
                    mk, MK = mkr.get()
                    pr_, PRR = prr.get()
                    oh, OH = ohr.get()
                    lgg = lg[:, :, 0:4]
                    S4 = [128, 4, 4]
                    S8 = [128, 4, 8]
                    DVE.op(lambda: V.reduce_max(w1[:, 0, :], lgg, axis=AX.X), outs=[W1], ins=[LG])
                    DVE.op(lambda: V.tensor_tensor(ohg[:], lgg, bc(w1[:, 0, :], S4, 2), op=ALU.is_equal), outs=[OHG], ins=[LG, W1])
                    DVE.op(lambda: V.tensor_tensor(d4[:], lgg, bc(w1[:, 0, :], S4, 2), op=ALU.subtract), outs=[D4], ins=[LG, W1])
                    ACT.op(lambda: A.activation(d4[:], d4[:], AF.Exp), outs=[D4], ins=[D4])
                    DVE.op(lambda: V.reduce_sum(w1[:, 1, :], d4[:], axis=AX.X), outs=[W1], ins=[D4])
                    DVE.op(lambda: V.reciprocal(w1[:, 2, :], w1[:, 1, :]), outs=[W1], ins=[W1])
                    lge = lg[:, :, 4:36].rearrange("p t (g e) -> p t g e", g=4)
                    p4 = pr_[:].rearrange("p t (g e) -> p t g e", g=4)
                    DVE.op(lambda: V.tensor_tensor(p4, lge, bc(ohg[:], [128, 4, 4, 8], 3), op=ALU.mult), outs=[PRR], ins=[LG, OHG])
                    DVE.op(lambda: V.reduce_sum(el[:, 0, :, :], pr_[:].rearrange("p t (g e) -> p t e g", g=4), axis=AX.X), outs=[EL], ins=[PRR])
                    DVE.op(lambda: V.reduce_max(w1[:, 3, :], el[:, 0, :, :], axis=AX.X), outs=[W1], ins=[EL])
                    DVE.op(lambda: V.tensor_tensor(mk[:, 0, :, :], el[:, 0, :, :], bc(w1[:, 3, :], S8, 2), op=ALU.is_equal), outs=[MK], ins=[EL, W1])
                    DVE.op(lambda: V.scalar_tensor_tensor(el[:, 1, :, :], mk[:, 0, :, :], -1e30, el[:, 0, :, :], op0=ALU.mult, op1=ALU.add), outs=[EL], ins=[MK, EL])
                    DVE.op(lambda: V.reduce_max(w1[:, 4, :], el[:, 1, :, :], axis=AX.X), outs=[W1], ins=[EL])
                    DVE.op(lambda: V.tensor_tensor(mk[:, 1, :, :], el[:, 1, :, :], bc(w1[:, 4, :], S8, 2), op=ALU.is_equal), outs=[MK], ins=[EL, W1])
                    DVE.op(lambda: V.tensor_tensor(w1[:, 5, :], w1[:, 4, :], w1[:, 3, :], op=ALU.subtract), outs=[W1], ins=[W1])
                    ACT.op(lambda: A.activation(w1[:, 6, :], w1[:, 5, :], AF.Exp), outs=[W1], ins=[W1])
                    DVE.op(lambda: V.tensor_scalar(w1[:, 6, :], w1[:, 6, :], 1.0, None, op0=ALU.add), outs=[W1], ins=[W1])
                    DVE.op(lambda: V.reciprocal(w1[:, 7, :], w1[:, 6, :]), outs=[W1], ins=[W1])
                    wt4 = wts[:, tb * 4:(tb + 1) * 4, :]
                    DVE.op(lambda: V.tensor_tensor(wt4[:, :, 0], w1[:, 7, :], w1[:, 2, :], op=ALU.mult), outs=[WT], ins=[W1])
                    DVE.op(lambda: V.tensor_tensor(wt4[:, :, 1], w1[:, 2, :], wt4[:, :, 0], op=ALU.subtract), outs=[WT], ins=[W1, WT])
                    for kk in range(2):
                        o4 = oh[:, kk, :, :].rearrange("p t (g e) -> p t g e", g=4)
                        DVE.op(lambda: V.tensor_tensor(o4, bc(ohg[:], [128, 4, 4, 8], 3), bc(mk[:, kk, :, :], [128, 4, 4, 8], 2), op=ALU.mult), outs=[OH], ins=[OHG, MK])
                    DVE.op(lambda: V.tensor_tensor(oh[:, 2, :, :], oh[:, 0, :, :], oh[:, 1, :, :], op=ALU.add), outs=[OH], ins=[OH])
                    o12, O12 = ohb.get()
                    POOL.op(lambda: G.tensor_copy(o12[:], oh[:, 2, :, :]), outs=[O12], ins=[OH])
                    rp, RP = prk.get()
                    mms = []
                    for ti in range(4):
                        mms.append((rp[:, ti, :], trib[:], o12[:, ti, :]))
                        for tj in range(ti):
                            mms.append((rp[:, ti, :], onesb[:], o12[:, tj, :]))
                    for tj in range(4):
                        mms.append((rp[:, 4, :], onesb[:], o12[:, tj, :]))
                    for i, (o_, l_, r_) in enumerate(mms):
                        PE.op(lambda: TE.matmul(o_, l_, r_, start=(i == 0), stop=(i == len(mms) - 1)), outs=[RP], ins=[TRIB, IDB, O12], inc=(i == len(mms) - 1))
                    rk, RK = rkr.get()
                    DVE.op(lambda: V.tensor_tensor(rk[:], rp[:, 0:4, :], bc(base[:], [128, 4, 32], 1), op=ALU.add), outs=[RK], ins=[RP, BASE])
                    DVE.op(lambda: V.tensor_tensor(base[:], base[:], rp[:, 4, :], op=ALU.add), outs=[BASE], ins=[BASE, RP])
                    for kk in range(2):
                        DVE.op(lambda: V.tensor_tensor(pr_[:], rk[:], oh[:, kk, :, :], op=ALU.mult), outs=[PRR], ins=[RK, OH])
                        DVE.op(lambda: V.reduce_sum(w1[:, 8 + kk, :], pr_[:], axis=AX.X), outs=[W1], ins=[PRR])
                    DVE.op(lambda: V.tensor_scalar(w1[:, 8:10, :], w1[:, 8:10, :], float(NSLOT - 1), None, op0=ALU.min), outs=[W1], ins=[W1])
                    sl4 = slots[:, tb * 4:(tb + 1) * 4, :]
                    DVE.op(lambda: V.tensor_copy(sl4.rearrange("p t k -> p k t"), w1[:, 8:10, :]), outs=[SL], ins=[W1])
                    for ti in range(4):
                        t = tb * 4 + ti
                        hb, HB = hbs[ti]
                        for kk in range(2):
                            POOL.dma(None, None, ins=[HB, SL], fn=lambda: G.indirect_dma_start(
                                out=xs_d, out_offset=bass.IndirectOffsetOnAxis(ap=slots[:, t, kk:kk + 1], axis=0), in_=hb[:], in_offset=None))
                SP.dma(cnt_d, base[:], ins=[BASE])
                K.barrier()
            K.scope = sc
            with ExitStack() as sc2:
                K.scope = sc2
                wgur = Rot(K, 2, [128, 8, D], BF16)
                wdr = Rot(K, 2, [128, 4, D], BF16)
                xsr = Rot(K, 3, [128, D], BF16)
                ptr = Rot(K, 2, [128, 8, 128], BF16, psum=True)
                psr = Rot(K, 5, [128, 512], F32, psum=True)
                xsT = Rot(K, 2, [128, 8, 512], BF16)
                slr = Rot(K, 2, [128, 512], F32)
                actT = Rot(K, 2, [128, 4, 512], BF16)
                yr = Rot(K, 3, [128, D], BF16)
                chunks = []
                c0 = 0
                while c0 < cap:
                    n = min(512, cap - c0)
                    chunks.append((c0, n))
                    c0 += n
                def load_w(e):
                    wgu, WGU = wgur.get()
                    wd, WD = wdr.get()
                    POOL.dma(wgu[:], kc(wgu_d[e]), outs=[WGU])
                    POOL.dma(wd[:], kc(wd_d[e]), outs=[WD])
                    return wgu, WGU, wd, WD
                items = [(e, c0, n) for e in range(32) for (c0, n) in chunks]

                def stage_a(e, c0, n):
                    xT, XT_ = xsT.get()
                    for j in range(n // 128):
                        r0 = e * cap + c0 + j * 128
                        xs, XS = xsr.get()
                        SP.dma(xs[:], xs_d[r0:r0 + 128, :], outs=[XS])
                        pt, PT = ptr.get()
                        transpose8(xs, XS, pt, PT, xT[:, :, j * 128:(j + 1) * 128], XT_, ACT if j % 2 else DVE)
                    return xT, XT_

                def stage_b(xT, XT_, wgu, WGU, n):
                    aT, AT = actT.get()
                    for f in range(4):
                        pg, PG = psr.get()
                        acc_mm(pg[:, 0:n], PG, [(wgu[:, k, f * 128:(f + 1) * 128], xT[:, k, 0:n]) for k in range(8)], [WGU, XT_])
                        pu, PU = psr.get()
                        acc_mm(pu[:, 0:n], PU, [(wgu[:, k, 512 + f * 128:512 + (f + 1) * 128], xT[:, k, 0:n]) for k in range(8)], [WGU, XT_])
                        sl_, SL_ = slr.get()
                        ACT.op(lambda: A.activation(sl_[:, 0:n], pg[:, 0:n], AF.Silu), outs=[SL_], ins=[PG])
                        DVE.op(lambda: V.tensor_tensor(aT[:, f, 0:n], sl_[:, 0:n], pu[:, 0:n], op=ALU.mult), outs=[AT], ins=[SL_, PU])
                    return aT, AT

                def stage_c(aT, AT, wd, WD, e, c0, n):
                    for j in range(n // 128):
                        r0 = e * cap + c0 + j * 128
                        y, Y = yr.get()
                        for hf in range(2):
                            po, PO = psr.get()
                            acc_mm(po[:], PO, [(aT[:, f, j * 128:(j + 1) * 128], wd[:, f, hf * 512:(hf + 1) * 512]) for f in range(4)], [AT, WD])
                            if hf:
                                ACT.op(lambda: A.copy(y[:, 512:1024], po[:]), outs=[Y], ins=[PO])
                            else:
                                DVE.op(lambda: V.tensor_copy(y[:, 0:512], po[:]), outs=[Y], ins=[PO])
                        SP.dma(ys_d[r0:r0 + 128, :], y[:], ins=[Y])

                wts_e = {0: load_w(0)}
                xa = stage_a(*items[0])
                for i, (e, c0, n) in enumerate(items):
                    if c0 == 0 and e + 1 < 32:
                        wts_e[e + 1] = load_w(e + 1)
                        wts_e.pop(e - 1, None)
                    wgu, WGU, wd, WD = wts_e[e]
                    xT, XT_ = xa
                    aT, AT = stage_b(xT, XT_, wgu, WGU, n)
                    if i + 1 < len(items):
                        xa = stage_a(*items[i + 1])
                    stage_c(aT, AT, wd, WD, e, c0, n)
                K.barrier()
            K.scope = sc
            with ExitStack() as sc3:
                K.scope = sc3
                xr = Rot(K, 4, [128, D], F32)
                y1r = Rot(K, 4, [128, D], BF16)
                y2r = Rot(K, 4, [128, D], BF16)

                def m3_load(t):
                    xt, XT = xr.get()
                    SP.dma(xt[:], out_d[t * 128:(t + 1) * 128, :], outs=[XT])
                    ys = []
                    for kk, rot in enumerate((y1r, y2r)):
                        y, Y = rot.get()
                        ys.append((y, Y))
                        POOL.dma(None, None, outs=[Y], ins=[SL], fn=lambda kk=kk, y=y: G.indirect_dma_start(
                            out=y[:], out_offset=None, in_=ys_d, in_offset=bass.IndirectOffsetOnAxis(ap=slots[:, t, kk:kk + 1], axis=0)))
                    return xt, XT, ys
                PF = 2
                loaded = [m3_load(t) for t in range(min(PF, NT))]
                for t in range(NT):
                    if t + PF < NT:
                        loaded.append(m3_load(t + PF))
                    xt, XT, ys = loaded.pop(0)
                    for kk in range(2):
                        y, Y = ys[kk]
                        DVE.op(lambda kk=kk, y=y: V.scalar_tensor_tensor(xt[:], y[:], wts[:, t, kk:kk + 1], xt[:], op0=ALU.mult, op1=ALU.add), outs=[XT], ins=[Y, WT, XT])
                    ACT.dma(out_d[t * 128:(t + 1) * 128, :], xt[:], ins=[XT])
                K.barrier()
        K.scope = K.es
    K.barrier()
    K.es.close()
    return nc


class _Views:
    def __init__(self, items):
        self.items = items
        self.i = 0

    def get(self):
        r = self.items[self.i]
        self.i = (self.i + 1) % len(self.items)
        return r


def _consts(cap):
    c = np.zeros((128, C_N), np.float32)
    c[:, C_IDF:C_IDF + 128] = np.eye(128, dtype=np.float32)
    c[:, C_ONES:C_ONES + 128] = 1.0
    for m in range(2):
        c[m * 64:(m + 1) * 64, C_BLK + m * 64:C_BLK + (m + 1) * 64] = 1.0
    for m in range(2):
        for d in range(8):
            c[m * 64 + d + 8, C_ROT + m * 64 + d] = -1.0
            c[m * 64 + d, C_ROT + m * 64 + d + 8] = 1.0
    c[:, C_TRI:C_TRI + 128] = np.triu(np.ones((128, 128), np.float32), k=1)
    inv_freq = np.exp(-math.log(THETA) * np.arange(HALF, dtype=np.float32) / HALF).astype(np.float32)
    for p in range(128):
        d = p % 64
        c[p, C_INVF] = inv_freq[d % 8] if d < ROT_DIMS else 0.0
    c[:, C_EOFF:C_EOFF + 32] = (np.arange(32, dtype=np.float32) * cap)[None, :]
    return c


def _pack(inp, cap):
    f = lambda a: np.ascontiguousarray(np.asarray(a, dtype=np.float32))
    pv = np.zeros((128, PV_N), np.float32)
    cw = f(inp["conv_w"])[0]
    pv[:, PV_CONVW:PV_CONVW + 32] = cw.reshape(4, 8, 128).transpose(2, 1, 0).reshape(128, 32)
    for off, name in ((PV_CONVB, "conv_b"), (PV_BA, "lru_ba"), (PV_BX, "lru_bx"), (PV_LAM, "lru_lambda")):
        pv[:, off:off + 8] = f(inp[name])[0].reshape(8, 128).T
    pv[:, PV_QN] = np.tile(f(inp["q_norm"])[0], 2)
    pv[:, PV_KN] = np.tile(f(inp["k_norm"])[0], 2)
    pv[:, PV_CQN] = f(inp["cq_norm"])[0]
    pv[:, PV_CKN] = f(inp["ck_norm"])[0]
    gv = np.zeros((GV_N,), np.float32)
    gv[GV_MIX:GV_MIX + D] = f(inp["norm_mix"])[0]
    gv[GV_CX:GV_CX + D] = f(inp["norm_cx"])[0]
    gv[GV_MEM:GV_MEM + D] = f(inp["norm_mem"])[0]
    gv[GV_FFN:GV_FFN + D] = f(inp["norm_ffn"])[0]
    gv[GV_SUBLN:GV_SUBLN + 128] = f(inp["subln"])[0]
    gv[GV_LAMV:GV_LAMV + 256] = np.concatenate([f(inp[n])[0] for n in ("lambda_q1", "lambda_k1", "lambda_q2", "lambda_k2")])
    gv[GV_BRG:GV_BRG + 36] = np.concatenate([f(inp["b_group"])[0], f(inp["b_router"])[0]])
    gvb = np.ascontiguousarray(np.broadcast_to(gv[None, :], (128, GV_N)))
    shared = {
        "w_in": f(inp["w_in"])[0], "pvec": pv, "gvec": gvb, "cst": _consts(cap),
        "lru_wa": f(inp["lru_wa"])[0], "lru_wx": f(inp["lru_wx"])[0],
        "w_lru_o": f(inp["w_lru_o"])[0], "w_attn_o": f(inp["w_attn_o"])[0], "w_out": f(inp["w_out"])[0],
        "w_cq": f(inp["w_cq"])[0], "w_ckv": f(inp["w_ckv"])[0], "w_co": f(inp["w_co"])[0],
        "w_rg": np.ascontiguousarray(np.concatenate([f(inp["w_group"])[0], f(inp["w_router"])[0]], axis=1)),
        "w_gate_up": f(inp["w_gate_up"])[0], "w_down": f(inp["w_down"])[0],
    }
    return shared


def run(inp, n_cores, nseq, S, cap, upto=99):
    nc = build(nseq, S, cap, upto)
    shared = _pack(inp, cap)
    x = np.asarray(inp["x"], np.float32)
    mem = np.asarray(inp["mem"], np.float32)
    pos = np.asarray(inp["positions"], np.int32)
    in_maps = []
    for c in range(n_cores):
        b0 = c * nseq
        m = dict(shared)
        m["x"] = np.ascontiguousarray(x[b0:b0 + nseq, :S].reshape(nseq * S, D))
        m["mem"] = np.ascontiguousarray(mem[b0:b0 + nseq].reshape(nseq * 256, D))
        m["pos"] = np.ascontiguousarray(pos[b0:b0 + nseq, :S])
        in_maps.append(m)
    res = run_bass_kernel_spmd(nc, in_maps, core_ids=list(range(n_cores)))
    outs = [np.asarray(r["out"]).reshape(nseq, S, D) for r in res.results]
    global LAST_CNT
    LAST_CNT = [np.asarray(r["cnt"])[0] for r in res.results] if "cnt" in res.results[0] else None
    return np.concatenate(outs, axis=0)


def kernel(**inputs):
    return run(inputs, N_CORES, 2, 4096, 896).astype(np.float32)
```
